# Optimizing a Trainium2 kernel written in Bass

```python
import math
import jax, jax.numpy as jnp
from jax import lax
import numpy as np

D_MODEL = 1024
BATCH = 16
SEQ = 2048
DEPTH = 4

N_A_LAYERS = DEPTH // 2
N_B_LAYERS = DEPTH - N_A_LAYERS
LRU_WIDTH = D_MODEL
LRU_BLOCKS = 8
LRU_BW = LRU_WIDTH // LRU_BLOCKS
CONV_WIDTH = 4
LRU_C = 8.0
N_HEADS = 16
HEAD_DIM = D_MODEL // N_HEADS
N_KV = 4
HPG = N_HEADS // N_KV
ROT_DIM = HEAD_DIM // 4
ROPE_THETA = 500000.0
CMP_BLOCK = 32
CMP_STRIDE = 16
CMP_HIDDEN = 4 * HEAD_DIM
SEL_BLOCK = 64
N_SEL = 8
WINDOW = 256
WIN_QBLOCK = 128
SEL_QCHUNK = 32
D_FF = 2816
N_EXPERTS = 8
TOP_K = 2
MOE_D_FF = 3584
DN_ALPHA = (2.0 * DEPTH) ** 0.25
DN_BETA = (8.0 * DEPTH) ** -0.25
LN_EPS = 1e-5
NEG = -1e30
BIG = 1e30
F32 = jnp.float32

kernel_name = 'hybrid_rglru_nsa_yoco_moe_deepnorm'


def layer_norm(x, g, b):
    xf = x.astype(F32)
    mu = jnp.mean(xf, -1, keepdims=True)
    var = jnp.mean(jnp.square(xf - mu), -1, keepdims=True)
    return ((xf - mu) * lax.rsqrt(var + LN_EPS) * g + b).astype(x.dtype)


def post_norm(x, y, g, b):
    return layer_norm(DN_ALPHA * x + y, g, b)


def partial_rope(x, pos):
    half = ROT_DIM // 2
    inv = ROPE_THETA ** (-jnp.arange(half, dtype=F32) / half)
    ang = pos.astype(F32)[:, None] * inv[None]
    shape = (1, x.shape[1]) + (1,) * (x.ndim - 3) + (half,)
    cos = jnp.cos(ang).reshape(shape)
    sin = jnp.sin(ang).reshape(shape)
    xr = x[..., :ROT_DIM].astype(F32)
    x1, x2 = xr[..., :half], xr[..., half:]
    rot = jnp.concatenate([x1 * cos - x2 * sin, x2 * cos + x1 * sin], -1).astype(x.dtype)
    return jnp.concatenate([rot, x[..., ROT_DIM:]], -1)


def causal_conv(x, w, b):
    S = x.shape[1]
    xp = jnp.pad(x, ((0, 0), (CONV_WIDTH - 1, 0), (0, 0)))
    y = b + w[0] * xp[:, 0:S]
    for k in range(1, CONV_WIDTH):
        y = y + w[k] * xp[:, k:k + S]
    return y


def recurrent_block(x, w_in, conv_w, conv_b, w_a, b_a, w_i, b_i, lam, w_out):
    B_, S, _ = x.shape
    u = x @ w_in
    gate = jax.nn.gelu(u[..., :LRU_WIDTH])
    xr = causal_conv(u[..., LRU_WIDTH:], conv_w, conv_b)
    xb = xr.reshape(B_, S, LRU_BLOCKS, LRU_BW)
    r = jax.nn.sigmoid(jnp.einsum('bsnc,ncd->bsnd', xb, w_a) + b_a).reshape(B_, S, LRU_WIDTH)
    i = jax.nn.sigmoid(jnp.einsum('bsnc,ncd->bsnd', xb, w_i) + b_i).reshape(B_, S, LRU_WIDTH)
    log_a = (-LRU_C * r.astype(F32)) * jax.nn.softplus(-lam.astype(F32))
    a = jnp.exp(log_a)
    b_t = jnp.sqrt(-jnp.expm1(2.0 * log_a)) * (i * xr).astype(F32)

    def combine(lhs, rhs):
        a1, h1 = lhs
        a2, h2 = rhs
        return a1 * a2, a2 * h1 + h2

    _, h = lax.associative_scan(combine, (a, b_t), axis=1)
    return (h.astype(x.dtype) * gate) @ w_out


def swiglu(x, w_gu, w_down):
    g, u = jnp.split(x @ w_gu, 2, axis=-1)
    return (jax.nn.silu(g) * u) @ w_down


def moe_swiglu(x, w_router, w_gu, w_down):
    logits = (x @ w_router).astype(F32)
    top_v, top_i = lax.top_k(logits, TOP_K)
    top_w = jax.nn.softmax(top_v, -1)
    gates = jnp.sum(jax.nn.one_hot(top_i, N_EXPERTS, dtype=F32) * top_w[..., None], -2)
    out = jnp.zeros_like(x)
    for e in range(N_EXPERTS):
        out = out + gates[..., e:e + 1].astype(x.dtype) * swiglu(x, w_gu[e], w_down[e])
    return out


def nsa_shared_kv(h, w_kv, cmp_pos, cmp_w1, cmp_w2):
    B_, S, _ = h.shape
    kv = (h @ w_kv).reshape(B_, S, 6, N_KV, HEAD_DIM)
    pos = jnp.arange(S)
    k_slc = partial_rope(kv[:, :, 2], pos)
    v_slc = kv[:, :, 3]
    k_win = partial_rope(kv[:, :, 4], pos)
    v_win = kv[:, :, 5]
    nc = (S - CMP_BLOCK) // CMP_STRIDE + 1
    idx = jnp.arange(nc)[:, None] * CMP_STRIDE + jnp.arange(CMP_BLOCK)[None]

    def compress(t, pe, w1, w2):
        blk = t[:, idx] + pe[None, None, :, None, :]
        blk = blk.transpose(0, 1, 3, 2, 4).reshape(B_, nc, N_KV, CMP_BLOCK * HEAD_DIM)
        return jax.nn.gelu(blk @ w1) @ w2

    k_cmp = compress(kv[:, :, 0], cmp_pos[0], cmp_w1[0], cmp_w2[0])
    v_cmp = compress(kv[:, :, 1], cmp_pos[1], cmp_w1[1], cmp_w2[1])
    return k_cmp, v_cmp, k_slc, v_slc, k_win, v_win


def cmp_branch(q, k_cmp, v_cmp, pos):
    nc = k_cmp.shape[1]
    s = jnp.einsum('bsghd,bcgd->bghsc', q, k_cmp).astype(F32)
    blk_end = jnp.arange(nc) * CMP_STRIDE + CMP_BLOCK - 1
    valid = blk_end[None, :] <= pos[:, None]
    p = jax.nn.softmax(jnp.where(valid, s, NEG), -1) * jnp.any(valid, -1)[:, None].astype(F32)
    o = jnp.einsum('bghsc,bcgd->bsghd', p.astype(v_cmp.dtype), v_cmp)
    return o, p


def select_blocks(p_cmp, pos):
    S = pos.shape[0]
    nb = S // SEL_BLOCK
    nc = p_cmp.shape[-1]
    c_start = jnp.arange(nc) * CMP_STRIDE
    j_start = jnp.arange(nb) * SEL_BLOCK
    overlap = ((c_start[:, None] < j_start[None] + SEL_BLOCK)
               & (c_start[:, None] + CMP_BLOCK > j_start[None])).astype(F32)
    imp = jnp.einsum('bghsc,cn->bsgn', p_cmp, overlap)
    cur = pos // SEL_BLOCK
    jj = jnp.arange(nb)
    forced = (jj[None] == 0) | (jj[None] == cur[:, None]) | (jj[None] == cur[:, None] - 1)
    causal = j_start[None] <= pos[:, None]
    score = jnp.where(forced[None, :, None, :], BIG,
                      jnp.where(causal[None, :, None, :], imp, NEG))
    _, idx = lax.top_k(score, min(N_SEL, nb))
    return idx


def slc_branch(q, k_slc, v_slc, idx, pos):
    B_, S = q.shape[:2]
    nb = S // SEL_BLOCK
    kb = k_slc.reshape(B_, nb, SEL_BLOCK, N_KV, HEAD_DIM).transpose(0, 3, 1, 2, 4)
    vb = v_slc.reshape(B_, nb, SEL_BLOCK, N_KV, HEAD_DIM).transpose(0, 3, 1, 2, 4)
    nq = S // SEL_QCHUNK
    kk = idx.shape[-1]
    bi = jnp.arange(B_)[:, None, None, None]
    gi = jnp.arange(N_KV)[None, None, :, None]

    def chunk(args):
        q_c, idx_c, t_c = args
        k_g = kb[bi, gi, idx_c]
        v_g = vb[bi, gi, idx_c]
        s = jnp.einsum('bqghd,bqgksd->bqghks', q_c, k_g).astype(F32)
        kpos = idx_c[..., None] * SEL_BLOCK + jnp.arange(SEL_BLOCK)
        mask = (kpos <= t_c[None, :, None, None, None])[:, :, :, None]
        s = jnp.where(mask, s, NEG)
        p = jax.nn.softmax(s.reshape(s.shape[:4] + (kk * SEL_BLOCK,)), -1).reshape(s.shape)
        return jnp.einsum('bqghks,bqgksd->bqghd', p.astype(v_g.dtype), v_g)

    qs = q.reshape(B_, nq, SEL_QCHUNK, N_KV, HPG, HEAD_DIM).swapaxes(0, 1)
    ids = idx.reshape(B_, nq, SEL_QCHUNK, N_KV, kk).swapaxes(0, 1)
    ts = pos.reshape(nq, SEL_QCHUNK)
    o = lax.map(chunk, (qs, ids, ts))
    return o.swapaxes(0, 1).reshape(B_, S, N_KV, HPG, HEAD_DIM)


def win_branch(q, k, v):
    B_, S = q.shape[:2]
    nqb = S // WIN_QBLOCK
    nback = WINDOW // WIN_QBLOCK

    def bands(t):
        tp = jnp.pad(t, ((0, 0), (WINDOW, 0), (0, 0), (0, 0)))
        tp = tp.reshape(B_, nqb + nback, WIN_QBLOCK, N_KV, HEAD_DIM)
        return jnp.concatenate([tp[:, j:j + nqb] for j in range(nback + 1)], axis=2)

    kw, vw = bands(k), bands(v)
    qb = q.reshape(B_, nqb, WIN_QBLOCK, N_KV, HPG, HEAD_DIM)
    s = jnp.einsum('bnighd,bnjgd->bnghij', qb, kw).astype(F32)
    i = jnp.arange(WIN_QBLOCK)[:, None]
    j = jnp.arange(WINDOW + WIN_QBLOCK)[None]
    rel = i + WINDOW - j
    kpos = jnp.arange(nqb)[:, None, None] * WIN_QBLOCK - WINDOW + j
    mask = (rel >= 0) & (rel < WINDOW) & (kpos >= 0)
    p = jax.nn.softmax(jnp.where(mask[None, :, None, None], s, NEG), -1)
    o = jnp.einsum('bnghij,bnjgd->bnighd', p.astype(vw.dtype), vw)
    return o.reshape(B_, S, N_KV, HPG, HEAD_DIM)


def nsa_mixer(x, w_qg, w_o, k_cmp, v_cmp, k_slc, v_slc, k_win, v_win):
    B_, S, _ = x.shape
    qg = x @ w_qg
    q = qg[..., :N_HEADS * HEAD_DIM].reshape(B_, S, N_KV, HPG, HEAD_DIM) * (HEAD_DIM ** -0.5)
    gates = jax.nn.sigmoid(qg[..., N_HEADS * HEAD_DIM:].astype(F32)).reshape(B_, S, N_KV, HPG, 3)
    pos = jnp.arange(S)
    q_rot = partial_rope(q, pos)
    o_cmp, p_cmp = cmp_branch(q, k_cmp, v_cmp, pos)
    idx = select_blocks(p_cmp, pos)
    o_slc = slc_branch(q_rot, k_slc, v_slc, idx, pos)
    o_win = win_branch(q_rot, k_win, v_win)
    o = (gates[..., 0:1] * o_cmp + gates[..., 1:2] * o_slc + gates[..., 2:3] * o_win).astype(x.dtype)
    return o.reshape(B_, S, N_HEADS * HEAD_DIM) @ w_o


def setup_inputs(seed: int = 0) -> dict:
    key = jax.random.key(seed)
    ks = jax.random.split(key, 26)

    def nrm(k, shape, scale):
        return jax.random.normal(k, shape, F32) * scale

    nA, nB = N_A_LAYERS, N_B_LAYERS
    n_dense = (DEPTH + 1) // 2
    n_moe = DEPTH // 2
    u = jax.random.uniform(ks[9], (nA, LRU_WIDTH), F32, 0.9, 0.999)
    s = u ** (1.0 / LRU_C)
    lam = jnp.log(s) - jnp.log1p(-s)
    return {
        'x': nrm(ks[0], (BATCH, SEQ, D_MODEL), 1.0),
        'ln_g': 1.0 + nrm(ks[1], (DEPTH, 2, D_MODEL), 0.02),
        'ln_b': nrm(ks[2], (DEPTH, 2, D_MODEL), 0.02),
        'lru_w_in': nrm(ks[3], (nA, D_MODEL, 2 * LRU_WIDTH), D_MODEL ** -0.5),
        'lru_conv_w': nrm(ks[4], (nA, CONV_WIDTH, LRU_WIDTH), CONV_WIDTH ** -0.5),
        'lru_conv_b': nrm(ks[5], (nA, LRU_WIDTH), 0.01),
        'lru_w_a': nrm(ks[6], (nA, LRU_BLOCKS, LRU_BW, LRU_BW), LRU_BW ** -0.5),
        'lru_b_a': nrm(ks[7], (nA, LRU_BLOCKS, LRU_BW), 0.01),
        'lru_w_i': nrm(ks[8], (nA, LRU_BLOCKS, LRU_BW, LRU_BW), LRU_BW ** -0.5),
        'lru_b_i': nrm(ks[10], (nA, LRU_BLOCKS, LRU_BW), 0.01),
        'lru_lambda': lam,
        'lru_w_out': nrm(ks[11], (nA, LRU_WIDTH, D_MODEL), LRU_WIDTH ** -0.5 * DN_BETA),
        'nsa_w_kv': nrm(ks[12], (D_MODEL, 6 * N_KV * HEAD_DIM), D_MODEL ** -0.5),
        'nsa_cmp_pos': nrm(ks[13], (2, CMP_BLOCK, HEAD_DIM), 0.1),
        'nsa_cmp_w1': nrm(ks[14], (2, CMP_BLOCK * HEAD_DIM, CMP_HIDDEN), (CMP_BLOCK * HEAD_DIM) ** -0.5),
        'nsa_cmp_w2': nrm(ks[15], (2, CMP_HIDDEN, HEAD_DIM), CMP_HIDDEN ** -0.5),
        'nsa_w_qg': nrm(ks[16], (nB, D_MODEL, N_HEADS * HEAD_DIM + 3 * N_HEADS), D_MODEL ** -0.5),
        'nsa_w_o': nrm(ks[17], (nB, N_HEADS * HEAD_DIM, D_MODEL), (N_HEADS * HEAD_DIM) ** -0.5 * DN_BETA),
        'ffn_w_gu': nrm(ks[18], (n_dense, D_MODEL, 2 * D_FF), D_MODEL ** -0.5),
        'ffn_w_down': nrm(ks[19], (n_dense, D_FF, D_MODEL), D_FF ** -0.5 * DN_BETA),
        'moe_w_router': nrm(ks[20], (n_moe, D_MODEL, N_EXPERTS), D_MODEL ** -0.5),
        'moe_w_gu': nrm(ks[21], (n_moe, N_EXPERTS, D_MODEL, 2 * MOE_D_FF), D_MODEL ** -0.5),
        'moe_w_down': nrm(ks[22], (n_moe, N_EXPERTS, MOE_D_FF, D_MODEL), MOE_D_FF ** -0.5 * DN_BETA),
    }


def reference(x, ln_g, ln_b, lru_w_in, lru_conv_w, lru_conv_b, lru_w_a, lru_b_a, lru_w_i, lru_b_i,
              lru_lambda, lru_w_out, nsa_w_kv, nsa_cmp_pos, nsa_cmp_w1, nsa_cmp_w2, nsa_w_qg, nsa_w_o,
              ffn_w_gu, ffn_w_down, moe_w_router, moe_w_gu, moe_w_down):
    h = x
    shared_kv = None
    for l in range(DEPTH):
        if l < N_A_LAYERS:
            y = recurrent_block(h, lru_w_in[l], lru_conv_w[l], lru_conv_b[l], lru_w_a[l], lru_b_a[l],
                                lru_w_i[l], lru_b_i[l], lru_lambda[l], lru_w_out[l])
        else:
            lb = l - N_A_LAYERS
            y = nsa_mixer(h, nsa_w_qg[lb], nsa_w_o[lb], *shared_kv)
        h = post_norm(h, y, ln_g[l, 0], ln_b[l, 0])
        if l % 2 == 0:
            f = swiglu(h, ffn_w_gu[l // 2], ffn_w_down[l // 2])
        else:
            f = moe_swiglu(h, moe_w_router[l // 2], moe_w_gu[l // 2], moe_w_down[l // 2])
        h = post_norm(h, f, ln_g[l, 1], ln_b[l, 1])
        if l == N_A_LAYERS - 1:
            shared_kv = nsa_shared_kv(h, nsa_w_kv, nsa_cmp_pos, nsa_cmp_w1, nsa_cmp_w2)
    return h
```

```python
import numpy as np
import concourse.bass as bass
import concourse.mybir as mybir

F32 = mybir.dt.float32
BF16 = mybir.dt.bfloat16
AF = mybir.ActivationFunctionType
ALU = mybir.AluOpType
AX = mybir.AxisListType

COMPUTE = ("pe", "act", "dve", "pool")
QUEUES = ("pe", "act", "dve", "pool", "sp")
SEM_CAP = 30000
import os as _os
STRICT_SAME = _os.environ.get('BASS_STRICT_SAME', '1') == '1'


class Dep:
    __slots__ = ("w", "r", "name")

    def __init__(self, name=""):
        self.w = None
        self.r = []
        self.name = name


class Op:
    __slots__ = ("q", "fn", "waits", "marked", "midx", "dma_sem", "dma_val", "idx")

    def __init__(self, q, fn):
        self.q = q
        self.fn = fn
        self.waits = []
        self.marked = False
        self.midx = None
        self.dma_sem = None
        self.dma_val = None


class Prog:
    def __init__(self, nc):
        self.nc = nc
        self.ops = {q: [] for q in QUEUES}
        self.dma_sems = []
        self.n_dma_sems = 0

    def new_dma_sem(self):
        self.dma_sems.append(0)
        return len(self.dma_sems) - 1

    def _collect(self, q, reads, writes):
        toks = []
        for d in reads:
            if d.w is not None:
                toks.append(d.w)
        for d in writes:
            if d.w is not None:
                toks.append(d.w)
            toks.extend(d.r)
        out = []
        seen = set()
        for t in toks:
            if t[0] == "c":
                o = t[1]
                if o.q == q:
                    if q in ("pe", "sp"):
                        continue
                    if not STRICT_SAME:
                        israw = any(d.w is t for d in reads)
                        if not israw:
                            continue
                key = ("c", id(o))
            else:
                key = t
            if key in seen:
                continue
            seen.add(key)
            out.append(t)
        return out

    def op(self, q, fn, reads=(), writes=()):
        o = Op(q, fn)
        o.waits = self._collect(q, reads, writes)
        for t in o.waits:
            if t[0] == "c":
                t[1].marked = True
        tok = ("c", o)
        for d in reads:
            d.r.append(tok)
        for d in writes:
            d.w = tok
            d.r = []
        self.ops[q].append(o)
        return o

    def dma(self, q, fn, sem, reads=(), writes=()):
        kind = "sw" if q == "pool" else "hw"
        if not hasattr(self, "_semkind"):
            self._semkind = {}
        key = (sem, kind)
        if key not in self._semkind:
            if any(k[0] == sem for k in self._semkind):
                self._semkind[key] = self.new_dma_sem()
            else:
                self._semkind[key] = sem
        sem = self._semkind[key]
        o = Op(q, fn)
        o.waits = self._collect(None, reads, writes)
        for t in o.waits:
            if t[0] == "c":
                t[1].marked = True
        self.dma_sems[sem] += 16
        o.dma_sem = sem
        o.dma_val = self.dma_sems[sem]
        tok = ("d", sem, o.dma_val)
        for d in reads:
            d.r.append(tok)
        for d in writes:
            d.w = tok
            d.r = []
        self.ops[q].append(o)
        return o

    def barrier(self):
        toks = []
        for q in COMPUTE:
            if self.ops[q]:
                for o in reversed(self.ops[q]):
                    if o.fn is not None and o.dma_sem is None:
                        toks.append(("c", o))
                        o.marked = True
                        break
        for s, v in enumerate(self.dma_sems):
            if v > 0:
                toks.append(("d", s, v))
        for q in QUEUES:
            o = Op(q, None)
            o.waits = [t for t in toks if not (t[0] == "c" and t[1].q == q)]
            self.ops[q].append(o)

    def replay(self, final_deps=()):
        nc = self.nc
        import contextlib
        nmark = {}
        for q in COMPUTE:
            m = 0
            for o in self.ops[q]:
                if o.marked:
                    o.midx = m
                    m += 1
            nmark[q] = m
        final_toks = []
        for d in final_deps:
            if d.w is not None:
                final_toks.append(d.w)
        with contextlib.ExitStack() as es:
            csems = {}
            for q in COMPUTE:
                n = max(1, (nmark[q] + SEM_CAP - 1) // SEM_CAP)
                csems[q] = [es.enter_context(nc.semaphore(f"c_{q}_{i}")) for i in range(n)]
            dsems = [es.enter_context(nc.semaphore(f"d_{i}")) for i in range(len(self.dma_sems))]
            block = es.enter_context(nc.Block())
            engs = {"pe": nc.tensor, "act": nc.scalar, "dve": nc.vector, "pool": nc.gpsimd, "sp": nc.sync}

            def emit_queue(q, eng):
                seen_c = {x: -1 for x in COMPUTE}
                seen_d = {}
                def do_wait(t):
                    if t[0] == "c":
                        o = t[1]
                        if o.midx <= seen_c[o.q]:
                            return
                        seen_c[o.q] = o.midx
                        eng.wait_ge(csems[o.q][o.midx // SEM_CAP], o.midx % SEM_CAP + 1)
                    else:
                        _, s, v = t
                        if seen_d.get(s, 0) >= v:
                            return
                        seen_d[s] = v
                        eng.wait_ge(dsems[s], v)
                for o in self.ops[q]:
                    for t in o.waits:
                        do_wait(t)
                    if o.fn is None:
                        continue
                    ins = o.fn()
                    if o.dma_sem is not None:
                        ins.then_inc(dsems[o.dma_sem], 16)
                    elif o.marked:
                        ins.then_inc(csems[q][o.midx // SEM_CAP], 1)
                if q == "sp":
                    for t in final_toks:
                        do_wait(t)

            @block.tensor
            def _(e):
                emit_queue("pe", e)

            @block.scalar
            def _(e):
                emit_queue("act", e)

            @block.vector
            def _(e):
                emit_queue("dve", e)

            @block.gpsimd
            def _(e):
                emit_queue("pool", e)

            @block.sync
            def _(e):
                emit_queue("sp", e)
        return {q: len(self.ops[q]) for q in QUEUES}

import os
from concourse.bass_utils import run_bass_kernel_spmd
import contextlib
import numpy as np

S = 2048
NS = 2
T = NS * S
D = 1024
ALPHA = float(8.0 ** 0.25)
EPS = 1e-5
DFF = 2816
MFF = 3584
NEG = -1e30

SP_LNG = 0
SP_LNB = 64
SP_CW = 128
SP_CB = 192
SP_BA = 208
SP_BI = 224
SP_LAM = 240


SP_PE = 256
NSP = 320

WSHAPES = {
    "lru_w_in": (2, 1024, 2048), "lru_w_a": (2, 8, 128, 128), "lru_w_i": (2, 8, 128, 128), "lru_w_out": (2, 1024, 1024),
    "nsa_w_kv": (1024, 1536), "nsa_cmp_w1": (2, 2048, 256), "nsa_cmp_w2": (2, 256, 64),
    "nsa_w_qg": (2, 1024, 1072), "nsa_w_o": (2, 1024, 1024),
    "ffn_w_gu": (2, 1024, 5632), "ffn_w_down": (2, 2816, 1024),
    "moe_w_router": (2, 1024, 8), "moe_w_gu": (2, 8, 1024, 7168), "moe_w_down": (2, 8, 3584, 1024),
}


def _make_consts():
    cols = {}
    parts = []
    off = 0

    def add(name, arr):
        nonlocal off
        arr = np.asarray(arr, np.float32)
        assert arr.shape[0] == 128
        cols[name] = (off, off + arr.shape[1])
        parts.append(arr)
        off += arr.shape[1]

    add("ones", np.full((128, 128), 1.0 / 1024.0))
    add("ident", np.eye(128))
    add("eps", np.full((128, 1), EPS))
    add("one", np.ones((128, 1)))
    add("zero", np.zeros((128, 1)))
    sel = np.zeros((128, 8, 128), np.float32)
    for e in range(8):
        sel[e, e, :] = 1.0
    add("sel", sel.reshape(128, 1024))
    RT = np.zeros((128, 128), np.float32)
    for hb_ in range(2):
        o = hb_ * 64
        for i in range(8):
            RT[o + i + 8, o + i] = -1.0
            RT[o + i, o + i + 8] = 1.0
    add("RT", RT)
    ii = np.arange(128)[:, None]
    jj = np.arange(128)[None, :]
    add("causal", np.where(jj <= ii, 0.0, NEG))
    jw = np.arange(384)[None, :]
    rel = ii + 256 - jw
    add("winmask", np.where((rel >= 0) & (rel < 256), 0.0, NEG))
    cm = np.full((128, 16, 128), NEG, np.float32)
    for qt in range(16):
        t = qt * 128 + np.arange(128)[:, None]
        c = np.arange(127)[None, :]
        cm[:, qt, 0:127] = np.where(c * 16 + 31 <= t, 0.0, NEG)
    add("cmpmask", cm.reshape(128, 2048))
    av = np.zeros((128, 16), np.float32)
    for qt in range(16):
        av[:, qt] = (qt * 128 + np.arange(128) >= 31).astype(np.float32)
    add("anyvalid", av)
    sm_ = np.zeros((128, 16, 32), np.float32)
    sa_ = np.zeros((128, 16, 32), np.float32)
    for qt in range(16):
        t = qt * 128 + np.arange(128)[:, None]
        n = np.arange(32)[None, :]
        cur = t // 64
        forced = (n == 0) | (n == cur) | (n == cur - 1)
        causal_ = (n * 64) <= t
        sm_[:, qt, :] = np.where(forced, 0.0, np.where(causal_, 1.0, 0.0))
        sa_[:, qt, :] = np.where(forced, 1e30, np.where(causal_, 0.0, NEG))
    add("selmul", sm_.reshape(128, 512))
    add("seladd", sa_.reshape(128, 512))
    ov = np.zeros((128, 32), np.float32)
    for c in range(127):
        for n in range(32):
            if (c * 16 < n * 64 + 64) and (c * 16 + 32 > n * 64):
                ov[c, n] = 1.0
    add("ovl", ov)
    if off % 2:
        add("pad", np.zeros((128, 1)))
    return np.concatenate(parts, axis=1), cols


CST_NP, CST_COLS = _make_consts()
NCST = CST_NP.shape[1]


def _make_cs_tab():
    half = 8
    inv = 500000.0 ** (-np.arange(half, dtype=np.float32) / half)
    pos = np.arange(S, dtype=np.float32)
    ang = pos[None, :] * inv[:, None]
    cos = np.ones((64, S), np.float32)
    sin = np.zeros((64, S), np.float32)
    cos[0:8] = np.cos(ang); cos[8:16] = np.cos(ang)
    sin[0:8] = np.sin(ang); sin[8:16] = np.sin(ang)
    tab = np.stack([np.concatenate([cos, cos], 0), np.concatenate([sin, sin], 0)], 0)
    return np.ascontiguousarray(tab.astype(np.float32))


CS_TAB = _make_cs_tab()


def _pack_small(inp):
    sp = np.zeros((128, NSP), np.float32)
    f = lambda a: np.asarray(a, np.float32)
    sp[:, SP_LNG:SP_LNG + 64] = f(inp["ln_g"]).reshape(4, 2, 8, 128).transpose(3, 0, 1, 2).reshape(128, 64)
    sp[:, SP_LNB:SP_LNB + 64] = f(inp["ln_b"]).reshape(4, 2, 8, 128).transpose(3, 0, 1, 2).reshape(128, 64)
    sp[:, SP_CW:SP_CW + 64] = f(inp["lru_conv_w"]).reshape(2, 4, 8, 128).transpose(3, 0, 2, 1).reshape(128, 64)
    sp[:, SP_CB:SP_CB + 16] = f(inp["lru_conv_b"]).reshape(2, 8, 128).transpose(2, 0, 1).reshape(128, 16)
    sp[:, SP_BA:SP_BA + 16] = f(inp["lru_b_a"]).transpose(2, 0, 1).reshape(128, 16)
    sp[:, SP_BI:SP_BI + 16] = f(inp["lru_b_i"]).transpose(2, 0, 1).reshape(128, 16)
    sp[:, SP_LAM:SP_LAM + 16] = f(inp["lru_lambda"]).reshape(2, 8, 128).transpose(2, 0, 1).reshape(128, 16)
    pe = f(inp["nsa_cmp_pos"]).transpose(2, 0, 1).reshape(64, 64)
    sp[0:64, SP_PE:SP_PE + 64] = pe
    sp[64:128, SP_PE:SP_PE + 64] = pe
    return sp


class Arena:
    def __init__(self, ap, n32):
        self.ap = ap
        self.n = n32
        self.off = 0
        self.marks = []

    def reset(self):
        self.off = 0
        self.marks = []

    def mark(self):
        self.marks.append(self.off)

    def release(self):
        self.off = self.marks.pop()

    def _take(self, n32):
        assert self.off + n32 <= self.n, f"arena overflow {self.off}+{n32}>{self.n}"
        a = self.ap[:, self.off:self.off + n32]
        self.off += n32
        return a

    def f32(self, shape):
        n = int(np.prod(shape[1:]))
        a = self._take(n)
        return self._shape(a, shape)

    def bf(self, shape):
        n = int(np.prod(shape[1:]))
        n32 = (n + 1) // 2
        a = self._take(n32).bitcast(BF16)
        if n32 * 2 != n:
            a = a[:, 0:n]
        return self._shape(a, shape)

    @staticmethod
    def _shape(a, shape):
        if len(shape) == 2:
            return a
        if len(shape) == 3:
            return a.rearrange("p (a b) -> p a b", a=shape[1])
        if len(shape) == 4:
            return a.rearrange("p (a b c) -> p a b c", a=shape[1], b=shape[2])
        raise ValueError


class Ctx:
    pass


def build_program(stop="all", dbg=False):
    nc = bass.Bass("TRN2", target_bir_lowering=False)
    P = Prog(nc)
    K = Ctx()
    K.nc = nc
    K.P = P

    def din(name, shape, dt=F32):
        return nc.dram_tensor(name, list(shape), dt, kind="ExternalInput").ap()

    xT = din("xT", [D, T])
    spk = din("spk", [128, NSP])
    cst = din("cst", [128, NCST])
    cs_tab = din("cs_tab", [2, 128, S])
    K.cs_tab = cs_tab
    W = {}
    for name, shape in WSHAPES.items():
        W[name] = din(name, shape)
    outT = nc.dram_tensor("outT", [D, T], F32, kind="ExternalOutput").ap()
    hres = nc.dram_tensor("hres", [D, T], F32, kind="Internal").ap()
    kvscr = {}
    kvscr["kT"] = nc.dram_tensor("kT_scr", [NS, 2, 2, 128, S], BF16, kind="Internal").ap()
    kvscr["v"] = nc.dram_tensor("v_scr", [NS, 2, 128, 16, 256], BF16, kind="Internal").ap()
    kvscr["kc"] = nc.dram_tensor("kc_scr", [NS, 2, 128, 128], BF16, kind="Internal").ap()
    kvscr["vc"] = nc.dram_tensor("vc_scr", [NS, 128, 256], BF16, kind="Internal").ap()

    es = contextlib.ExitStack()
    with es:
        NA = 49152
        arena_t = es.enter_context(nc.sbuf_tensor("arena", [128, NA], F32))
        NPERS = NSP + NCST + 64 + 16
        A = Arena(arena_t[:], NA - NPERS)
        spk_t = arena_t[:, NA - NSP:NA]
        cst_t = arena_t[:, NA - NSP - NCST:NA - NSP]
        identb = arena_t[:, NA - NPERS:NA - NPERS + 64].bitcast(BF16)
        ovlb = arena_t[:, NA - NPERS + 64:NA - NPERS + 80].bitcast(BF16)
        K.identb = identb
        K.ovlb = ovlb
        K.final = []
        psS = es.enter_context(nc.psum_tensor("psS", [128, 2048], F32))
        psA = es.enter_context(nc.psum_tensor("psA", [128, 512], F32))
        psB = es.enter_context(nc.psum_tensor("psB", [128, 512], F32))
        psT = [es.enter_context(nc.psum_tensor(f"psT{i}", [128, 1024], BF16)) for i in range(2)]
        K.bank = [psS[:, i * 512:(i + 1) * 512] for i in range(4)] + [psA[:], psB[:]]
        K.dbank = [Dep(f"bank{i}") for i in range(6)]
        K.psS = psS
        K.psT = [psT[0][:], psT[1][:]]
        K.dpsT = [Dep("psT0"), Dep("psT1")]

        sem_pool = {}

        def sem(role, i=0, n=1):
            key = (role, i % n)
            if key not in sem_pool:
                sem_pool[key] = P.new_dma_sem()
            return sem_pool[key]

        d_spk = Dep("spk")
        d_cst = Dep("cst")
        P.dma("sp", lambda: nc.sync.dma_start(out=spk_t, in_=spk[:, :]), sem("spk"), writes=[d_spk])
        P.dma("sp", lambda: nc.sync.dma_start(out=cst_t, in_=cst[:, :]), sem("cst"), writes=[d_cst])
        _ia, _ib = CST_COLS["ident"]
        _oa, _ob = CST_COLS["ovl"]
        P.op("act", lambda: nc.scalar.copy(out=identb, in_=cst_t[:, _ia:_ib]), reads=[d_cst], writes=[Dep()])
        P.op("act", lambda: nc.scalar.copy(out=ovlb, in_=cst_t[:, _oa:_ob]), reads=[d_cst], writes=[Dep()])
        P.barrier()

        def spc(col, n=1):
            return spk_t[:, col:col + n]

        def cc(name):
            a, b = CST_COLS[name]
            return cst_t[:, a:b]

        ones_f = cc("ones")
        ident_f = cc("ident")
        eps_c = cc("eps")
        one_c = cc("one")
        zero_c = cc("zero")

        K.A = A

        def load_h_bf16(dst, src, t0, nt, dep, role, i=0):
            srcap = src.rearrange("(c p) t -> p c t", p=128)[:, :, t0:t0 + nt]
            P.dma("pool", lambda: nc.gpsimd.dma_start(out=dst, in_=srcap), sem(role, i, 2), writes=[dep])

        def postnorm_tile(src, dst, t0, nt, yfn, l, i, bufs, ddst_extra=None):
            R, dR, O, dO, SQ, dSQ, MS, dMS = bufs
            gcol = SP_LNG + (l * 2 + i) * 8
            bcol = SP_LNB + (l * 2 + i) * 8
            srcap = src.rearrange("(c p) t -> p c t", p=128)[:, :, t0:t0 + nt]
            dstap = dst.rearrange("(c p) t -> p c t", p=128)[:, :, t0:t0 + nt]
            P.dma("sp", lambda: nc.sync.dma_start(out=R[:, :, 0:nt], in_=srcap), sem("pnR"), writes=[dR])
            pm, dpm = K.bank[0][:, 0:nt], K.dbank[0]
            pq, dpq = K.bank[1][:, 0:nt], K.dbank[1]
            for c in range(8):
                yap, ydeps = yfn(c)
                Rc = R[:, c, 0:nt]
                P.op("dve", lambda Rc=Rc, yap=yap: nc.vector.scalar_tensor_tensor(out=Rc, in0=Rc, scalar=ALPHA, in1=yap, op0=ALU.mult, op1=ALU.add),
                     reads=[dR] + ydeps, writes=[dR])
                sq = SQ[c % 2][:, 0:nt]
                dsq = dSQ[c % 2]
                P.op("act", lambda Rc=Rc, sq=sq: nc.scalar.activation(out=sq, in_=Rc, func=AF.Square), reads=[dR], writes=[dsq])
                P.op("pe", lambda Rc=Rc, c=c: nc.tensor.matmul(pm, ones_f, Rc, start=(c == 0), stop=(c == 7)), reads=[dR], writes=[dpm])
                P.op("pe", lambda sq=sq, c=c: nc.tensor.matmul(pq, ones_f, sq, start=(c == 0), stop=(c == 7)), reads=[dsq], writes=[dpq])
            mean = MS[:, 0, 0:nt]
            rstd = MS[:, 1, 0:nt]
            P.op("act", lambda: nc.scalar.copy(out=mean, in_=pm), reads=[dpm], writes=[dMS])
            P.op("dve", lambda: nc.vector.tensor_tensor(out=rstd, in0=mean, in1=mean, op=ALU.mult), reads=[dMS], writes=[dMS])
            P.op("dve", lambda: nc.vector.tensor_tensor(out=rstd, in0=pq, in1=rstd, op=ALU.subtract), reads=[dMS, dpq], writes=[dMS])
            P.op("act", lambda: nc.scalar.activation(out=rstd, in_=rstd, func=AF.Sqrt, bias=eps_c, scale=1.0), reads=[dMS], writes=[dMS])
            P.op("dve", lambda: nc.vector.reciprocal(out=rstd, in_=rstd), reads=[dMS], writes=[dMS])
            for c in range(8):
                Rc = R[:, c, 0:nt]
                Oc = O[:, c, 0:nt]
                P.op("dve", lambda Rc=Rc: nc.vector.tensor_tensor(out=Rc, in0=Rc, in1=mean, op=ALU.subtract), reads=[dR, dMS], writes=[dR])
                P.op("dve", lambda Rc=Rc: nc.vector.tensor_tensor(out=Rc, in0=Rc, in1=rstd, op=ALU.mult), reads=[dR, dMS], writes=[dR])
                P.op("act", lambda Rc=Rc, Oc=Oc, c=c: nc.scalar.activation(out=Oc, in_=Rc, func=AF.Identity, bias=spc(bcol + c), scale=spc(gcol + c)),
                     reads=[dR, d_spk], writes=[dO])
            dd = Dep("dst")
            P.dma("sp", lambda: nc.sync.dma_start(out=dstap, in_=O[:, :, 0:nt]), sem("pnO"), reads=[dO], writes=[dd])
            if dst is outT:
                K.final.append(dd)
            return dd

        def alloc_pn(nt):
            R = A.f32([128, 8, nt]); O = A.f32([128, 8, nt])
            SQ = [A.f32([128, nt]) for _ in range(2)]
            MS = A.f32([128, 2, nt])
            return (R, Dep("R"), O, Dep("O"), SQ, [Dep("sq0"), Dep("sq1")], MS, Dep("MS"))

        def lru_phase(l, src, dst):
            P.barrier()
            A.reset()
            wout = A.bf([128, 8, 1024]); d_wout = Dep()
            P.dma("pool", lambda: nc.gpsimd.dma_start(out=wout, in_=W["lru_w_out"][l].rearrange("(j p) n -> p j n", p=128)), sem("w0"), writes=[d_wout])
            wa = A.bf([128, 8, 128]); wi = A.bf([128, 8, 128]); d_wa = Dep(); d_wi = Dep()
            P.dma("pool", lambda: nc.gpsimd.dma_start(out=wa, in_=W["lru_w_a"][l].rearrange("j c d -> c j d")), sem("w1"), writes=[d_wa])
            P.dma("pool", lambda: nc.gpsimd.dma_start(out=wi, in_=W["lru_w_i"][l].rearrange("j c d -> c j d")), sem("w2"), writes=[d_wi])
            hb = A.bf([128, 8, S]); d_hb = Dep()
            ysb = A.bf([128, 8, S]); d_ys = [Dep() for _ in range(8)]
            wgx = [A.bf([128, 8, 2, 128]) for _ in range(2)]; d_wgx = [Dep(), Dep()]
            sp8 = A.f32([128, 8]); d_sp8 = Dep()
            lam = spc(SP_LAM + l * 8, 8)
            P.op("act", lambda: nc.scalar.activation(out=sp8, in_=lam, func=AF.Exp, scale=-1.0), reads=[d_spk], writes=[d_sp8])
            P.op("act", lambda: nc.scalar.activation(out=sp8, in_=sp8, func=AF.Ln, bias=one_c, scale=1.0), reads=[d_sp8], writes=[d_sp8])
            P.op("dve", lambda: nc.vector.tensor_scalar(out=sp8, in0=sp8, scalar1=-8.0, scalar2=None, op0=ALU.mult), reads=[d_sp8], writes=[d_sp8])
            w_in_v = W["lru_w_in"][l].rearrange("(kc p) (two n) -> p kc two n", p=128, two=2)
            for s in range(NS):
                load_h_bf16(hb, src, s * S, S, d_hb, "hb")
                A.mark()
                G = A.f32([128, S]); XP = A.f32([128, S + 8]); XR = A.f32([128, S]); RA = A.f32([128, S])
                IB = A.f32([128, S]); T1 = A.f32([128, S]); H = A.f32([128, S]); xrb = A.bf([128, S])
                dG, dXP, dXR, dRA, dIB, dT1, dH, dxrb = [Dep(n) for n in "G XP XR RA IB T1 H xrb".split()]
                P.op("pool", lambda XP=XP: nc.gpsimd.memset(XP[:, 0:3], 0.0), writes=[dXP])
                bi = 0
                for j in range(8):
                    wt = wgx[j % 2]; dwt = d_wgx[j % 2]
                    P.dma("pool", lambda wt=wt, j=j: nc.gpsimd.dma_start(out=wt[:, :, 0, :], in_=w_in_v[:, :, 0, j * 128:(j + 1) * 128]), sem("wgx", j, 2), writes=[dwt])
                    P.dma("pool", lambda wt=wt, j=j: nc.gpsimd.dma_start(out=wt[:, :, 1, :], in_=w_in_v[:, :, 1, j * 128:(j + 1) * 128]), sem("wgx", j, 2), reads=[dwt], writes=[dwt])
                    for tt in range(4):
                        sl = slice(tt * 512, (tt + 1) * 512)
                        for which in range(2):
                            b = bi % 4; bi += 1
                            pb, dpb = K.bank[b], K.dbank[b]
                            for kc in range(8):
                                P.op("pe", lambda pb=pb, wt=wt, kc=kc, which=which, sl=sl: nc.tensor.matmul(pb, wt[:, kc, which, :], hb[:, kc, sl], start=(kc == 0), stop=(kc == 7)),
                                     reads=[dwt, d_hb], writes=[dpb])
                            if which == 0:
                                P.op("act", lambda pb=pb, sl=sl, G=G: nc.scalar.activation(out=G[:, sl], in_=pb, func=AF.Gelu_apprx_tanh), reads=[dpb], writes=[dG])
                            else:
                                P.op("dve", lambda pb=pb, tt=tt, XP=XP: nc.vector.tensor_copy(out=XP[:, 3 + tt * 512:3 + (tt + 1) * 512], in_=pb), reads=[dpb], writes=[dXP])
                    cw = SP_CW + (l * 8 + j) * 4
                    cb = SP_CB + l * 8 + j
                    P.op("dve", lambda XP=XP, XR=XR, cw=cw, cb=cb: nc.vector.tensor_scalar(out=XR, in0=XP[:, 0:S], scalar1=spc(cw), scalar2=spc(cb), op0=ALU.mult, op1=ALU.add),
                         reads=[dXP, d_spk], writes=[dXR])
                    for k in range(1, 4):
                        P.op("dve", lambda XP=XP, XR=XR, cw=cw, k=k: nc.vector.scalar_tensor_tensor(out=XR, in0=XP[:, k:k + S], scalar=spc(cw + k), in1=XR, op0=ALU.mult, op1=ALU.add),
                             reads=[dXP, dXR, d_spk], writes=[dXR])
                    P.op("act", lambda XR=XR, xrb=xrb: nc.scalar.copy(out=xrb, in_=XR), reads=[dXR], writes=[dxrb])
                    for tt in range(4):
                        sl = slice(tt * 512, (tt + 1) * 512)
                        for which in range(2):
                            b = bi % 4; bi += 1
                            pb, dpb = K.bank[b], K.dbank[b]
                            wmat, dw = (wa, d_wa) if which == 0 else (wi, d_wi)
                            bcol = (SP_BA if which == 0 else SP_BI) + l * 8 + j
                            dstt, ddst = (RA, dRA) if which == 0 else (IB, dIB)
                            P.op("pe", lambda pb=pb, wmat=wmat, j=j, sl=sl, xrb=xrb: nc.tensor.matmul(pb, wmat[:, j, :], xrb[:, sl], start=True, stop=True), reads=[dw, dxrb], writes=[dpb])
                            P.op("act", lambda pb=pb, dstt=dstt, sl=sl, bcol=bcol: nc.scalar.activation(out=dstt[:, sl], in_=pb, func=AF.Sigmoid, bias=spc(bcol), scale=1.0),
                                 reads=[dpb, d_spk], writes=[ddst])
                    P.op("act", lambda RA=RA, j=j: nc.scalar.activation(out=RA, in_=RA, func=AF.Exp, scale=sp8[:, j:j + 1]), reads=[dRA, d_sp8], writes=[dRA])
                    P.op("dve", lambda RA=RA, T1=T1: nc.vector.tensor_tensor(out=T1, in0=RA, in1=RA, op=ALU.mult), reads=[dRA], writes=[dT1])
                    P.op("dve", lambda T1=T1: nc.vector.tensor_scalar(out=T1, in0=T1, scalar1=-1.0, scalar2=1.0, op0=ALU.mult, op1=ALU.add), reads=[dT1], writes=[dT1])
                    P.op("act", lambda T1=T1: nc.scalar.activation(out=T1, in_=T1, func=AF.Sqrt, bias=zero_c, scale=1.0), reads=[dT1], writes=[dT1])
                    P.op("dve", lambda IB=IB, XR=XR: nc.vector.tensor_tensor(out=IB, in0=IB, in1=XR, op=ALU.mult), reads=[dIB, dXR], writes=[dIB])
                    P.op("dve", lambda IB=IB, T1=T1: nc.vector.tensor_tensor(out=IB, in0=IB, in1=T1, op=ALU.mult), reads=[dIB, dT1], writes=[dIB])
                    P.op("dve", lambda RA=RA, IB=IB, H=H: nc.vector.tensor_tensor_scan(out=H, data0=RA, data1=IB, initial=0.0, op0=ALU.mult, op1=ALU.add), reads=[dRA, dIB], writes=[dH])
                    P.op("dve", lambda H=H, G=G, j=j: nc.vector.tensor_tensor(out=ysb[:, j, :], in0=H, in1=G, op=ALU.mult), reads=[dH, dG], writes=[d_ys[j]])
                P.barrier()
                A.release()
                A.mark()
                bufs = alloc_pn(512)
                yb = 0
                for tt in range(4):
                    t0 = s * S + tt * 512

                    def yfn(c, tt=tt):
                        nonlocal yb
                        b = 4 + (yb % 2); yb += 1
                        pb, dpb = K.bank[b], K.dbank[b]
                        for j in range(8):
                            P.op("pe", lambda pb=pb, j=j, c=c, tt=tt: nc.tensor.matmul(pb, wout[:, j, c * 128:(c + 1) * 128], ysb[:, j, tt * 512:(tt + 1) * 512], start=(j == 0), stop=(j == 7)),
                                 reads=[d_wout, d_ys[j]], writes=[dpb])
                        return pb, [dpb]
                    postnorm_tile(src, dst, t0, 512, yfn, l, 0, bufs)
                P.barrier()
                A.release()

        def ffn_phase(l, src, dst, moe):
            P.barrier()
            A.reset()
            li = l // 2
            NT = 1024
            if moe:
                units = [(e, f0, 4) for e in range(8) for f0 in range(0, 28, 4)]
                wgu_of = lambda e: W["moe_w_gu"][li, e]
                wd_of = lambda e: W["moe_w_down"][li, e]
            else:
                units = [(0, f0, min(4, 22 - f0)) for f0 in range(0, 22, 4)]
                wgu_of = lambda e: W["ffn_w_gu"][li]
                wd_of = lambda e: W["ffn_w_down"][li]
            hb_ = [A.bf([128, 8, NT]) for _ in range(2)]; d_hb_ = [Dep(), Dep()]
            acc = A.f32([128, 8, NT]); d_acc = [Dep() for _ in range(2)]
            wgu_s = [A.bf([128, 8, 2, 512]) for _ in range(2)]; d_wgu = [Dep(), Dep()]
            wd_s = [A.bf([128, 4, 1024]) for _ in range(2)]; d_wd = [Dep(), Dep()]
            act = [A.bf([128, 4, 512]) for _ in range(2)]; d_act = [Dep(), Dep()]
            sg = [A.f32([128, 512]) for _ in range(2)]; d_sg = [Dep(), Dep()]
            bufs = alloc_pn(256)
            if moe:
                gb = A.f32([128, 2, 512]); d_gb = Dep()
                gatesT_ = [A.f32([128, NT]) for _ in range(2)]; d_gT_ = [Dep(), Dep()]
                Rr = [A.f32([128, 8, 128]) for _ in range(2)]; d_Rr = [Dep(), Dep()]
                wr = A.f32([128, 8, 8]); d_wr = Dep()
                P.dma("sp", lambda: nc.sync.dma_start(out=wr, in_=W["moe_w_router"][li].rearrange("(kc p) e -> p kc e", p=128)), sem("w0"), writes=[d_wr])
                lg = A.f32([128, 8]); mx8 = A.f32([128, 8]); nm = A.f32([128, 1]); ex = A.f32([128, 8]); msk = A.f32([128, 8]); den = A.f32([128, 1])
                d_r = Dep("router")
            rcount = [0]

            def prep(st):
                t0 = st * NT
                hb, d_hb = hb_[st % 2], d_hb_[st % 2]
                load_h_bf16(hb, src, t0, NT, d_hb, "hb", st)
                if not moe:
                    return
                gatesT, d_gT = gatesT_[st % 2], d_gT_[st % 2]
                for sub in range(8):
                    ri = rcount[0] % 2; rcount[0] += 1
                    R, dR = Rr[ri], d_Rr[ri]
                    srcap = src.rearrange("(c p) t -> p c t", p=128)[:, :, t0 + sub * 128:t0 + (sub + 1) * 128]
                    P.dma("sp", lambda R=R, srcap=srcap: nc.sync.dma_start(out=R, in_=srcap), sem("rr", ri, 2), writes=[dR])
                    pl, dpl = K.bank[4][:, 0:8], K.dbank[4]
                    for kc in range(8):
                        P.op("pe", lambda kc=kc, R=R: nc.tensor.matmul(pl, R[:, kc, :], wr[:, kc, :], start=(kc == 0), stop=(kc == 7)),
                             reads=[dR, d_wr], writes=[dpl])
                    P.op("dve", lambda: nc.vector.tensor_copy(out=lg, in_=pl), reads=[dpl], writes=[d_r])
                    P.op("dve", lambda: nc.vector.max(out=mx8, in_=lg), reads=[d_r], writes=[d_r])
                    P.op("dve", lambda: nc.vector.tensor_scalar(out=nm, in0=mx8[:, 0:1], scalar1=-1.0, scalar2=None, op0=ALU.mult), reads=[d_r], writes=[d_r])
                    P.op("act", lambda: nc.scalar.activation(out=ex, in_=lg, func=AF.Exp, bias=nm, scale=1.0), reads=[d_r], writes=[d_r])
                    P.op("dve", lambda: nc.vector.tensor_scalar(out=msk, in0=lg, scalar1=mx8[:, 1:2], scalar2=None, op0=ALU.is_ge), reads=[d_r], writes=[d_r])
                    P.op("dve", lambda: nc.vector.tensor_tensor(out=ex, in0=ex, in1=msk, op=ALU.mult), reads=[d_r], writes=[d_r])
                    P.op("dve", lambda: nc.vector.reduce_sum(out=den, in_=ex, axis=AX.X), reads=[d_r], writes=[d_r])
                    P.op("dve", lambda: nc.vector.reciprocal(out=den, in_=den), reads=[d_r], writes=[d_r])
                    P.op("dve", lambda: nc.vector.tensor_scalar(out=ex, in0=ex, scalar1=den, scalar2=None, op0=ALU.mult), reads=[d_r], writes=[d_r])
                    pt, dpt = K.bank[5][0:8, 0:128], K.dbank[5]
                    P.op("pe", lambda: nc.tensor.transpose(pt, ex, ident_f), reads=[d_r, d_cst], writes=[dpt])
                    c0 = sub * 128
                    P.op("act", lambda c0=c0, gatesT=gatesT: nc.scalar.copy(out=gatesT[0:8, c0:c0 + 128], in_=pt), reads=[dpt], writes=[d_gT])

            ui = 0
            prep(0)
            nst = T // NT
            for st in range(nst):
                t0 = st * NT
                hb, d_hb = hb_[st % 2], d_hb_[st % 2]
                if moe:
                    gatesT, d_gT = gatesT_[st % 2], d_gT_[st % 2]
                bi = 0
                di = 0
                cur_e = -1
                for uidx, (e, f0, fcu) in enumerate(units):
                    if uidx == (len(units) * 2) // 3 and st + 1 < nst:
                        prep(st + 1)
                    slot = ui % 2; ui += 1
                    wg_t, dwg = wgu_s[slot], d_wgu[slot]
                    wd_t, dwd = wd_s[slot], d_wd[slot]
                    wsrc = wgu_of(e).rearrange("(kc p) (two f) -> p kc two f", p=128, two=2)[:, :, :, f0 * 128:(f0 + fcu) * 128]
                    P.dma("pool", lambda wg_t=wg_t, wsrc=wsrc, fcu=fcu: nc.gpsimd.dma_start(out=wg_t[:, :, 0, 0:fcu * 128], in_=wsrc[:, :, 0, :]), sem("wgu", slot, 2), writes=[dwg])
                    P.dma("pool", lambda wg_t=wg_t, wsrc=wsrc, fcu=fcu: nc.gpsimd.dma_start(out=wg_t[:, :, 1, 0:fcu * 128], in_=wsrc[:, :, 1, :]), sem("wgu", slot, 2), reads=[dwg], writes=[dwg])
                    dsrc = wd_of(e)[f0 * 128:(f0 + fcu) * 128, :].rearrange("(fc p) n -> p fc n", p=128)
                    P.dma("pool", lambda wd_t=wd_t, dsrc=dsrc, fcu=fcu: nc.gpsimd.dma_start(out=wd_t[:, 0:fcu, :], in_=dsrc), sem("wd", slot, 2), writes=[dwd])
                    if moe and e != cur_e:
                        cur_e = e
                        for tt in range(2):
                            pb, dpb = K.bank[4], K.dbank[4]
                            P.op("pe", lambda pb=pb, e=e, tt=tt, gatesT=gatesT: nc.tensor.matmul(pb, cc("sel")[0:8, e * 128:(e + 1) * 128], gatesT[0:8, tt * 512:(tt + 1) * 512], start=True, stop=True),
                                 reads=[d_gT, d_cst], writes=[dpb])
                            P.op("act", lambda pb=pb, tt=tt: nc.scalar.copy(out=gb[:, tt, :], in_=pb), reads=[dpb], writes=[d_gb])
                    for tt in range(2):
                        sl = slice(tt * 512, (tt + 1) * 512)
                        at, dat = act[tt], d_act[tt]
                        for fc in range(fcu):
                            bg = bi % 2; bu = 2 + bi % 2; bi += 1
                            pg, dpg = K.bank[bg], K.dbank[bg]
                            pu, dpu = K.bank[bu], K.dbank[bu]
                            for kc in range(8):
                                P.op("pe", lambda pg=pg, wg_t=wg_t, kc=kc, fc=fc, sl=sl, hb=hb: nc.tensor.matmul(pg, wg_t[:, kc, 0, fc * 128:(fc + 1) * 128], hb[:, kc, sl], start=(kc == 0), stop=(kc == 7)),
                                     reads=[dwg, d_hb], writes=[dpg])
                            for kc in range(8):
                                P.op("pe", lambda pu=pu, wg_t=wg_t, kc=kc, fc=fc, sl=sl, hb=hb: nc.tensor.matmul(pu, wg_t[:, kc, 1, fc * 128:(fc + 1) * 128], hb[:, kc, sl], start=(kc == 0), stop=(kc == 7)),
                                     reads=[dwg, d_hb], writes=[dpu])
                            sgt, dsgt = sg[bi % 2], d_sg[bi % 2]
                            P.op("act", lambda pg=pg, sgt=sgt: nc.scalar.activation(out=sgt, in_=pg, func=AF.Silu), reads=[dpg], writes=[dsgt])
                            if moe:
                                P.op("dve", lambda sgt=sgt, tt=tt: nc.vector.tensor_tensor(out=sgt, in0=sgt, in1=gb[:, tt, :], op=ALU.mult), reads=[dsgt, d_gb], writes=[dsgt])
                            P.op("dve", lambda sgt=sgt, pu=pu, at=at, fc=fc: nc.vector.tensor_tensor(out=at[:, fc, :], in0=sgt, in1=pu, op=ALU.mult), reads=[dsgt, dpu], writes=[dat])
                    for tt in range(2):
                        sl = slice(tt * 512, (tt + 1) * 512)
                        at, dat = act[tt], d_act[tt]
                        for c in range(8):
                            b = 4 + di % 2; di += 1
                            pd, dpd = K.bank[b], K.dbank[b]
                            for fc in range(fcu):
                                P.op("pe", lambda pd=pd, wd_t=wd_t, fc=fc, c=c, at=at, fcu=fcu: nc.tensor.matmul(pd, wd_t[:, fc, c * 128:(c + 1) * 128], at[:, fc, :], start=(fc == 0), stop=(fc == fcu - 1)),
                                     reads=[dwd, dat], writes=[dpd])
                            if uidx == 0:
                                P.op("dve", lambda pd=pd, c=c, sl=sl: nc.vector.tensor_copy(out=acc[:, c, sl], in_=pd), reads=[dpd], writes=[d_acc[tt]])
                            else:
                                P.op("dve", lambda pd=pd, c=c, sl=sl: nc.vector.tensor_tensor(out=acc[:, c, sl], in0=acc[:, c, sl], in1=pd, op=ALU.add), reads=[dpd, d_acc[tt]], writes=[d_acc[tt]])
                for k4 in range(4):
                    def yfn(c, k4=k4):
                        return acc[:, c, k4 * 256:(k4 + 1) * 256], [d_acc[k4 // 2]]
                    postnorm_tile(src, dst, t0 + k4 * 256, 256, yfn, l, 1, bufs)

        K.lru_phase = lru_phase
        K.ffn_phase = ffn_phase
        K.postnorm_tile = postnorm_tile
        K.alloc_pn = alloc_pn
        K.load_h_bf16 = load_h_bf16
        K.sem = sem
        K.cc = cc
        K.spc = spc
        K.d_cst = d_cst
        K.d_spk = d_spk
        K.W = W
        K.kvscr = kvscr

        seq = [("lru0", lambda d: lru_phase(0, xT, d)),
               ("ffn0", lambda d: ffn_phase(0, hres, d, False)),
               ("lru1", lambda d: lru_phase(1, hres, d)),
               ("ffn1", lambda d: ffn_phase(1, hres, d, True)),
               ("nsa2", lambda d: (kv_phase(K, hres), nsa_phase(K, 2, hres, d))),
               ("ffn2", lambda d: ffn_phase(2, hres, d, False)),
               ("nsa3", lambda d: nsa_phase(K, 3, hres, d)),
               ("all", lambda d: ffn_phase(3, hres, d, True))]
        if stop == "nsaonly":
            seq = [("nsaonly", lambda d: nsa_phase(K, 2, xT, d))]
        for name, fn in seq:
            if name == stop:
                fn(outT)
                break
            fn(hres)
        counts = P.replay(K.final)
        print("op counts", counts, "dma sems", len(P.dma_sems), "final", len(K.final))
    return nc

def kv_phase(K, src):
    nc, P, A, W = K.nc, K.P, K.A, K.W
    sem, cc, spc = K.sem, K.cc, K.spc
    P.barrier()
    A.reset()
    wkv = A.bf([128, 8, 1536]); d_wkv = Dep()
    P.dma("pool", lambda: nc.gpsimd.dma_start(out=wkv, in_=W["nsa_w_kv"].rearrange("(kc p) n -> p kc n", p=128)), sem("w0"), writes=[d_wkv])
    w1 = A.bf([128, 2, 32, 256]); d_w1 = [Dep() for _ in range(4)]
    for tkv in range(2):
        for half in range(2):
            P.dma("pool", lambda tkv=tkv, half=half: nc.gpsimd.dma_start(out=w1[half * 64:(half + 1) * 64, tkv, :, :], in_=W["nsa_cmp_w1"][tkv].rearrange("(l d) m -> d l m", d=64)),
                  sem("w1", tkv * 2 + half, 4), writes=[d_w1[tkv * 2 + half]])
    w2k = A.bf([128, 2, 128]); d_w2k = [Dep(), Dep()]
    for half in range(2):
        P.dma("pool", lambda half=half: nc.gpsimd.dma_start(out=w2k[:, :, half * 64:(half + 1) * 64], in_=W["nsa_cmp_w2"][0].rearrange("(mc p) d -> p mc d", p=128)),
              sem("w2k", half, 2), writes=[d_w2k[half]])
    w2v = A.bf([128, 2, 64]); d_w2v = Dep()
    P.dma("pool", lambda: nc.gpsimd.dma_start(out=w2v, in_=W["nsa_cmp_w2"][1].rearrange("(mc p) d -> p mc d", p=128)), sem("w2v"), writes=[d_w2v])
    peb = A.bf([128, 64]); d_peb = Dep()
    P.op("act", lambda: nc.scalar.copy(out=peb, in_=spc(SP_PE, 64)), reads=[K.d_spk], writes=[d_peb])
    biasH = A.f32([128, 4]); d_bH = Dep()
    RT = cc("RT")
    for tkv in range(2):
        for mc in range(2):
            pb, dpb = K.bank[4 + (mc % 2)][:, 0:1], K.dbank[4 + (mc % 2)]
            for l in range(32):
                P.op("pe", lambda pb=pb, tkv=tkv, mc=mc, l=l: nc.tensor.matmul(pb, w1[0:64, tkv, l, mc * 128:(mc + 1) * 128], peb[0:64, tkv * 32 + l:tkv * 32 + l + 1], start=(l == 0), stop=(l == 31)),
                     reads=[d_w1[tkv * 2], d_peb], writes=[dpb])
            P.op("act", lambda pb=pb, tkv=tkv, mc=mc: nc.scalar.copy(out=biasH[:, tkv * 2 + mc:tkv * 2 + mc + 1], in_=pb), reads=[dpb], writes=[d_bH])
    hb = A.bf([128, 8, S]); d_hb = Dep()
    cos_t = A.f32([128, S]); sin_t = A.f32([128, S]); d_cs = [Dep(), Dep()]
    P.dma("sp", lambda: nc.sync.dma_start(out=cos_t, in_=K.cs_tab[0]), sem("cs", 0, 2), writes=[d_cs[0]])
    P.dma("sp", lambda: nc.sync.dma_start(out=sin_t, in_=K.cs_tab[1]), sem("cs", 1, 2), writes=[d_cs[1]])
    kraw = [A.f32([128, 512]) for _ in range(2)]; d_kraw = [Dep(), Dep()]
    k1 = [A.f32([128, 512]) for _ in range(2)]; d_k1 = [Dep(), Dep()]
    kT_sb = [A.bf([128, S]) for _ in range(2)]; d_kT = [Dep(), Dep()]
    v_sb = A.bf([128, 16, 256]); d_v = Dep()
    csrc = A.bf([128, S]); d_csrc = Dep()
    hid = A.bf([128, 2, 128]); d_hid = Dep()
    kc_sb = A.bf([128, 128]); d_kc = Dep()
    vc_sb = A.bf([128, 256]); d_vc = Dep()
    P.op("pool", lambda: nc.gpsimd.memset(kc_sb, 0.0), writes=[d_kc])
    P.op("pool", lambda: nc.gpsimd.memset(vc_sb, 0.0), writes=[d_vc])
    bi = 0
    it = 0
    for s in range(NS):
        K.load_h_bf16(hb, src, s * S, S, d_hb, "hb")
        for ti, typ in enumerate((2, 4)):
            for gp in range(2):
                col0 = typ * 256 + gp * 128
                kt, dkt = kT_sb[it % 2], d_kT[it % 2]; it += 1
                for tt in range(4):
                    sl = slice(tt * 512, (tt + 1) * 512)
                    b = 4 + bi % 2; bi += 1
                    pb, dpb = K.bank[b], K.dbank[b]
                    for kc in range(8):
                        P.op("pe", lambda pb=pb, kc=kc, col0=col0, sl=sl: nc.tensor.matmul(pb, wkv[:, kc, col0:col0 + 128], hb[:, kc, sl], start=(kc == 0), stop=(kc == 7)),
                             reads=[d_wkv, d_hb], writes=[dpb])
                    kr, dkr = kraw[tt % 2], d_kraw[tt % 2]
                    kk, dkk = k1[tt % 2], d_k1[tt % 2]
                    P.op("act", lambda kr=kr, pb=pb: nc.scalar.copy(out=kr, in_=pb), reads=[dpb], writes=[dkr])
                    b2 = 4 + bi % 2; bi += 1
                    pr, dpr = K.bank[b2], K.dbank[b2]
                    P.op("pe", lambda pr=pr, kr=kr: nc.tensor.matmul(pr, RT, kr, start=True, stop=True), reads=[dkr, K.d_cst], writes=[dpr])
                    P.op("dve", lambda kk=kk, kr=kr, sl=sl: nc.vector.tensor_tensor(out=kk, in0=kr, in1=cos_t[:, sl], op=ALU.mult), reads=[dkr, d_cs[0]], writes=[dkk])
                    P.op("dve", lambda kr=kr, pr=pr, sl=sl: nc.vector.tensor_tensor(out=kr, in0=pr, in1=sin_t[:, sl], op=ALU.mult), reads=[dpr, d_cs[1]], writes=[dkr])
                    P.op("dve", lambda kt=kt, kk=kk, kr=kr, sl=sl: nc.vector.tensor_tensor(out=kt[:, sl], in0=kk, in1=kr, op=ALU.add), reads=[dkk, dkr], writes=[dkt])
                dst = K.kvscr["kT"][s, ti, gp]
                P.dma("sp", lambda kt=kt, dst=dst: nc.sync.dma_start(out=dst, in_=kt), sem("kvst", it, 2), reads=[dkt], writes=[Dep()])
        for ti, typ in enumerate((3, 5)):
            for kc16 in range(16):
                b = 4 + bi % 2; bi += 1
                pb, dpb = K.bank[b][:, 0:256], K.dbank[b]
                for kc in range(8):
                    P.op("pe", lambda pb=pb, kc=kc, kc16=kc16, typ=typ: nc.tensor.matmul(pb, hb[:, kc, kc16 * 128:(kc16 + 1) * 128], wkv[:, kc, typ * 256:(typ + 1) * 256], start=(kc == 0), stop=(kc == 7)),
                         reads=[d_wkv, d_hb], writes=[dpb])
                P.op("act", lambda pb=pb, kc16=kc16: nc.scalar.copy(out=v_sb[:, kc16, :], in_=pb), reads=[dpb], writes=[d_v])
            dst = K.kvscr["v"][s, ti]
            P.dma("sp", lambda dst=dst: nc.sync.dma_start(out=dst, in_=v_sb), sem("kvst2"), reads=[d_v], writes=[Dep()])
        for tkv in range(2):
            for gp in range(2):
                col0 = tkv * 256 + gp * 128
                for tt in range(4):
                    sl = slice(tt * 512, (tt + 1) * 512)
                    b = 4 + bi % 2; bi += 1
                    pb, dpb = K.bank[b], K.dbank[b]
                    for kc in range(8):
                        P.op("pe", lambda pb=pb, kc=kc, col0=col0, sl=sl: nc.tensor.matmul(pb, wkv[:, kc, col0:col0 + 128], hb[:, kc, sl], start=(kc == 0), stop=(kc == 7)),
                             reads=[d_wkv, d_hb], writes=[dpb])
                    P.op("act", lambda pb=pb, sl=sl: nc.scalar.copy(out=csrc[:, sl], in_=pb), reads=[dpb], writes=[d_csrc])
                for half in range(2):
                    g = gp * 2 + half
                    base = half * 64
                    for mc in range(2):
                        b = 4 + bi % 2; bi += 1
                        pb, dpb = K.bank[b][:, 0:127], K.dbank[b]
                        for l in range(32):
                            P.op("pe", lambda pb=pb, base=base, tkv=tkv, l=l, mc=mc: nc.tensor.matmul(pb, w1[base:base + 64, tkv, l, mc * 128:(mc + 1) * 128], csrc[base:base + 64, l:l + 16 * 126 + 1:16], start=(l == 0), stop=(l == 31)),
                                 reads=[d_w1[tkv * 2 + half], d_csrc], writes=[dpb])
                        P.op("act", lambda pb=pb, mc=mc, tkv=tkv: nc.scalar.activation(out=hid[:, mc, 0:127], in_=pb, func=AF.Gelu_apprx_tanh, bias=biasH[:, tkv * 2 + mc:tkv * 2 + mc + 1], scale=1.0),
                             reads=[dpb, d_bH], writes=[d_hid])
                    b = 4 + bi % 2; bi += 1
                    if tkv == 0:
                        pb, dpb = K.bank[b][:, 0:127], K.dbank[b]
                        for mc in range(2):
                            P.op("pe", lambda pb=pb, mc=mc: nc.tensor.matmul(pb, w2k[:, mc, :], hid[:, mc, 0:127], start=(mc == 0), stop=(mc == 1)), reads=d_w2k + [d_hid], writes=[dpb])
                        P.op("act", lambda pb=pb, base=base: nc.scalar.copy(out=kc_sb[base:base + 64, 0:127], in_=pb[base:base + 64, :]), reads=[dpb], writes=[d_kc])
                    else:
                        pb, dpb = K.bank[b][0:127, 0:64], K.dbank[b]
                        for mc in range(2):
                            P.op("pe", lambda pb=pb, mc=mc: nc.tensor.matmul(pb, hid[:, mc, 0:127], w2v[:, mc, :], start=(mc == 0), stop=(mc == 1)), reads=[d_w2v, d_hid], writes=[dpb])
                        P.op("act", lambda pb=pb, g=g: nc.scalar.copy(out=vc_sb[0:127, g * 64:(g + 1) * 64], in_=pb), reads=[dpb], writes=[d_vc])
                if tkv == 0:
                    dst = K.kvscr["kc"][s, gp]
                    P.dma("sp", lambda dst=dst: nc.sync.dma_start(out=dst, in_=kc_sb), sem("kvst3"), reads=[d_kc], writes=[Dep()])
            if tkv == 1:
                dst = K.kvscr["vc"][s]
                P.dma("sp", lambda dst=dst: nc.sync.dma_start(out=dst, in_=vc_sb), sem("kvst4"), reads=[d_vc], writes=[Dep()])


def nsa_phase(K, l, src, dst):
    nc, P, A, W = K.nc, K.P, K.A, K.W
    sem, cc, spc = K.sem, K.cc, K.spc
    lb = l - 2
    P.barrier()
    A.reset()
    d_cst = K.d_cst
    wq = A.bf([128, 8, 8, 128]); d_wq = []
    wq_src = W["nsa_w_qg"][lb][:, 0:1024].rearrange("(kc p) (g hh d) -> p kc g hh d", p=128, g=4, hh=4)
    for gp in range(2):
        for half in range(2):
            for hh in range(4):
                if not d_wq:
                    d_wq.append(Dep())
                P.dma("pool", lambda gp=gp, half=half, hh=hh: nc.gpsimd.dma_start(out=wq[:, :, gp * 4 + hh, half * 64:(half + 1) * 64], in_=wq_src[:, :, gp * 2 + half, hh, :]),
                      sem("w1", 0, 4), writes=[d_wq[0]])
    wgt = A.bf([128, 8, 48]); d_wgt = Dep()
    P.dma("pool", lambda: nc.gpsimd.dma_start(out=wgt, in_=W["nsa_w_qg"][lb][:, 1024:1072].rearrange("(kc p) n -> p kc n", p=128)), sem("w2v"), writes=[d_wgt])
    wo = A.bf([128, 8, 1024]); d_wo = Dep()
    P.dma("pool", lambda: nc.gpsimd.dma_start(out=wo, in_=W["nsa_w_o"][lb].rearrange("(j p) n -> p j n", p=128)), sem("w0"), writes=[d_wo])
    kT = A.bf([128, 2, 2, S]); d_kTl = Dep()
    v = A.bf([128, 2, 16, 256]); d_vl = Dep()
    kc = A.bf([128, 2, 128]); d_kcl = Dep()
    vc = A.bf([128, 256]); d_vcl = Dep()
    hb2 = [A.bf([128, 8, 256]) for _ in range(2)]; d_hb2 = [Dep(), Dep()]
    cs2 = [A.f32([128, 2, 256]) for _ in range(2)]; d_cs2 = [Dep(), Dep()]
    qraw = [A.f32([128, 256]) for _ in range(2)]; d_qraw = [Dep(), Dep()]
    qt1 = [A.f32([128, 256]) for _ in range(2)]; d_qt1 = [Dep(), Dep()]
    qb = A.bf([128, 8, 256]); d_qb = Dep()
    qr = A.bf([128, 8, 256]); d_qr = Dep()
    gts_ = [A.f32([128, 48]) for _ in range(2)]; d_gts_ = [Dep(), Dep()]
    oacc_ = [A.f32([128, 1024]) for _ in range(2)]; d_oacc_ = [[Dep() for _ in range(16)] for _ in range(2)]
    ob = A.bf([128, 1024]); d_ob = Dep()
    oT_ = [A.bf([128, 8, 256]) for _ in range(2)]; d_oT_ = [Dep(), Dep()]
    _sc = A.f32([128, 4, 128]); SC_ = [_sc, _sc]; _dsc = Dep(); d_SC_ = [_dsc, _dsc]
    _pcx = A.f32([128, 4, 128]); PCX_ = [_pcx, _pcx]; _dpcx = [Dep() for _ in range(4)]; d_PCX_ = [_dpcx, _dpcx]
    _pn = A.bf([128, 4, 128]); pn_ = [_pn, _pn]; _dpn = Dep(); d_pn_ = [_dpn, _dpn]
    _ptc = A.bf([128, 4, 128]); pTc_ = [_ptc, _ptc]; _dptc = Dep(); d_pTc_ = [_dptc, _dptc]
    st4_ = [A.f32([128, 16]) for _ in range(2)]
    d_mx4_ = [Dep(), Dep()]; d_ss4_ = [[Dep() for _ in range(4)] for _ in range(2)]; d_rs4_ = [Dep(), Dep()]
    score_ = [A.f32([128, 32]) for _ in range(2)]; top8_ = [A.f32([128, 8]) for _ in range(2)]; selb_ = [A.f32([128, 32]) for _ in range(2)]
    d_score_ = [Dep(), Dep()]; d_selb_ = [Dep(), Dep()]
    NBUF = 2
    SM = [A.f32([128, S]) for _ in range(NBUF)]; d_SM = [Dep() for _ in range(NBUF)]
    pbf = [A.bf([128, S]) for _ in range(NBUF)]; d_pbf = [Dep() for _ in range(NBUF)]
    pT = [A.bf([128, 16, 128]) for _ in range(NBUF)]; d_pT = [Dep() for _ in range(NBUF)]
    st1 = [A.f32([128, 4]) for _ in range(4)]
    d_mx1 = [Dep() for _ in range(4)]; d_ss1 = [Dep() for _ in range(4)]; d_gs1 = [Dep() for _ in range(4)]
    bufs = K.alloc_pn(256)
    identb = K.identb
    ovlb = K.ovlb
    RT = cc("RT")
    causal = cc("causal")
    winmask = cc("winmask")
    cmpmask = cc("cmpmask").rearrange("p (q c) -> p q c", q=16)
    anyvalid = cc("anyvalid")
    selmul = cc("selmul").rearrange("p (q n) -> p q n", q=16)
    seladd = cc("seladd").rearrange("p (q n) -> p q n", q=16)
    bk4, dbk4 = K.bank[4], K.dbank[4]
    bk5 = K.bank[5]
    d_po = [K.dbank[5]] * 3
    d_poc = K.dbank[5]
    d_pim = K.dbank[5]
    bi = 0
    ti_ = 0
    cnt = {"it": 0}

    def nb():
        nonlocal bi
        b = 4 + bi % 2; bi += 1
        if b == 5:
            return K.bank[5], [K.dbank[5]]
        return K.bank[4], [K.dbank[4]]

    def nT():
        nonlocal ti_
        b = ti_ % 2; ti_ += 1
        return K.psT[b], K.dpsT[b]

    def batch_pre(s, q2):
        tq = s * S + q2 * 256
        h2, dh2 = hb2[q2 % 2], d_hb2[q2 % 2]
        K.load_h_bf16(h2, src, tq, 256, dh2, "hb2", q2)
        c2, dc2 = cs2[q2 % 2], d_cs2[q2 % 2]
        P.dma("sp", lambda c2=c2, q2=q2: nc.sync.dma_start(out=c2, in_=K.cs_tab[:, :, q2 * 256:(q2 + 1) * 256].rearrange("two p t -> p two t")), sem("cs", q2, 2), writes=[dc2])
        for cp in range(8):
            pq, dpq = nb()
            pq = pq[:, 0:256]
            for kc_ in range(8):
                P.op("pe", lambda pq=pq, kc_=kc_, cp=cp, h2=h2: nc.tensor.matmul(pq, wq[:, kc_, cp, :], h2[:, kc_, :], start=(kc_ == 0), stop=(kc_ == 7)),
                     reads=d_wq + [dh2], writes=dpq)
            qw, dqw = qraw[cp % 2], d_qraw[cp % 2]
            q1, dq1 = qt1[cp % 2], d_qt1[cp % 2]
            P.op("act", lambda qw=qw, pq=pq: nc.scalar.mul(out=qw, in_=pq, mul=0.125), reads=dpq, writes=[dqw])
            P.op("act", lambda qw=qw, cp=cp: nc.scalar.copy(out=qb[:, cp, :], in_=qw), reads=[dqw], writes=[d_qb])
            pr, dpr = nb()
            pr = pr[:, 0:256]
            P.op("pe", lambda pr=pr, qw=qw: nc.tensor.matmul(pr, RT, qw, start=True, stop=True), reads=[dqw, d_cst], writes=dpr)
            P.op("dve", lambda q1=q1, qw=qw, c2=c2: nc.vector.tensor_tensor(out=q1, in0=qw, in1=c2[:, 0, :], op=ALU.mult), reads=[dqw, dc2], writes=[dq1])
            P.op("dve", lambda qw=qw, pr=pr, c2=c2: nc.vector.tensor_tensor(out=qw, in0=pr, in1=c2[:, 1, :], op=ALU.mult), reads=dpr + [dc2], writes=[dqw])
            P.op("dve", lambda q1=q1, qw=qw, cp=cp: nc.vector.tensor_tensor(out=qr[:, cp, :], in0=q1, in1=qw, op=ALU.add), reads=[dq1, dqw], writes=[d_qr])

    def tile_pre(s, q2, sub):
        h2, dh2 = hb2[q2 % 2], d_hb2[q2 % 2]
        ts = slice(sub * 128, (sub + 1) * 128)
        gts, d_gts = gts_[sub], d_gts_[sub]
        pg, dpg = nb()
        pg = pg[:, 0:48]
        for kc_ in range(8):
            P.op("pe", lambda pg=pg, kc_=kc_, h2=h2, ts=ts: nc.tensor.matmul(pg, h2[:, kc_, ts], wgt[:, kc_, :], start=(kc_ == 0), stop=(kc_ == 7)), reads=[dh2, d_wgt], writes=dpg)
        P.op("act", lambda pg=pg, gts=gts: nc.scalar.activation(out=gts, in_=pg, func=AF.Sigmoid), reads=dpg, writes=[d_gts])

    def tile_post(s, q2, sub):
        ts = slice(sub * 128, (sub + 1) * 128)
        oacc, d_oacc = oacc_[sub], d_oacc_[sub]
        oT, d_oT = oT_[q2 % 2], d_oT_[q2 % 2]
        P.op("act", lambda oacc=oacc: nc.scalar.copy(out=ob, in_=oacc), reads=d_oacc, writes=[d_ob])
        pt_, dpt_ = nT()
        for c in range(8):
            P.op("pe", lambda pt_=pt_, c=c: nc.tensor.transpose(pt_[:, c * 128:(c + 1) * 128], ob[:, c * 128:(c + 1) * 128], identb), reads=[d_ob, d_cst], writes=[dpt_])
        P.op("act", lambda pt_=pt_, ts=ts, oT=oT: nc.scalar.copy(out=oT[:, :, ts], in_=pt_.rearrange("p (c t) -> p c t", c=8)), reads=[dpt_], writes=[d_oT])

    def batch_post(s, q2):
        tq = s * S + q2 * 256
        oT, d_oT = oT_[q2 % 2], d_oT_[q2 % 2]

        def yfn(c):
            pb, dpb = nb()
            pb = pb[:, 0:256]
            for j in range(8):
                P.op("pe", lambda pb=pb, j=j, c=c: nc.tensor.matmul(pb, wo[:, j, c * 128:(c + 1) * 128], oT[:, j, :], start=(j == 0), stop=(j == 7)), reads=[d_wo, d_oT], writes=dpb)
            return pb, dpb
        K.postnorm_tile(src, dst, tq, 256, yfn, l, 0, bufs)

    def cmp_item(q2, sub, g):
        qt = q2 * 2 + sub
        ts = slice(sub * 128, (sub + 1) * 128)
        gp = g // 2
        base = (g % 2) * 64
        p = g % 2
        SC, d_SC = SC_[p], d_SC_[p]
        PCX, d_PCX = PCX_[p], d_PCX_[p]
        pn, d_pn = pn_[p], d_pn_[p]
        pTc, d_pTc = pTc_[p], d_pTc_[p]
        st4 = st4_[p]; d_mx4 = d_mx4_[p]; d_ss4 = d_ss4_[p]; d_rs4 = d_rs4_[p]
        score, top8, selb = score_[p], top8_[p], selb_[p]
        d_score, d_selb = d_score_[p], d_selb_[p]
        gts, d_gts = gts_[sub], d_gts_[sub]
        oacc, d_oacc = oacc_[sub], d_oacc_[sub]
        gts3 = gts.rearrange("p (h b) -> p h b", b=3)

        def stA():
            pc = bk4
            for hh in range(4):
                cp = gp * 4 + hh
                P.op("pe", lambda hh=hh, cp=cp: nc.tensor.matmul(pc[:, hh * 128:hh * 128 + 127], qb[base:base + 64, cp, ts], kc[base:base + 64, gp, 0:127], start=True, stop=True),
                     reads=[d_qb, d_kcl], writes=[dbk4])
            pc3 = pc.rearrange("p (h c) -> p h c", h=4)
            P.op("dve", lambda: nc.vector.tensor_tensor(out=SC[:, :, 0:127], in0=pc3[:, :, 0:127], in1=cmpmask[:, qt, 0:127].unsqueeze(1).to_broadcast([128, 4, 127]), op=ALU.add),
                 reads=[dbk4, d_cst], writes=[d_SC])
            P.op("dve", lambda: nc.vector.tensor_reduce(out=st4[:, 0:4], in_=SC[:, :, 0:127], axis=AX.X, op=ALU.max), reads=[d_SC], writes=[d_mx4])
            P.op("dve", lambda: nc.vector.tensor_scalar(out=st4[:, 4:8], in0=st4[:, 0:4], scalar1=-1.0, scalar2=None, op0=ALU.mult), reads=[d_mx4], writes=[d_mx4])

        def stB():
            for hh in range(4):
                P.op("act", lambda hh=hh: nc.scalar.activation(out=PCX[:, hh, 0:127], in_=SC[:, hh, 0:127], func=AF.Exp, bias=st4[:, 4 + hh:5 + hh], scale=1.0, accum_out=st4[:, 8 + hh:9 + hh]),
                     reads=[d_SC, d_mx4], writes=[d_PCX[hh], d_ss4[hh]])
            P.op("dve", lambda: nc.vector.reciprocal(out=st4[:, 12:16], in_=st4[:, 8:12]), reads=d_ss4, writes=[d_rs4])
            if qt == 0:
                P.op("dve", lambda: nc.vector.tensor_scalar(out=st4[:, 12:16], in0=st4[:, 12:16], scalar1=anyvalid[:, 0:1], scalar2=None, op0=ALU.mult), reads=[d_rs4, d_cst], writes=[d_rs4])
            P.op("dve", lambda: nc.vector.tensor_tensor(out=pn[:, :, 0:127], in0=PCX[:, :, 0:127], in1=st4[:, 12:16].unsqueeze(2).to_broadcast([128, 4, 127]), op=ALU.mult),
                 reads=d_PCX + [d_rs4], writes=[d_pn])
            pt_, dpt_ = nT()
            for hh in range(4):
                P.op("pe", lambda pt_=pt_, hh=hh: nc.tensor.transpose(pt_[0:127, hh * 128:(hh + 1) * 128], pn[:, hh, 0:127], identb), reads=[d_pn, d_cst], writes=[dpt_])
            P.op("act", lambda pt_=pt_: nc.scalar.copy(out=pTc[0:127, :, :], in_=pt_[0:127, 0:512].rearrange("p (h t) -> p h t", h=4)), reads=[dpt_], writes=[d_pTc])

        def stC():
            po = bk5[:, 256:512]
            for hh in range(4):
                P.op("pe", lambda hh=hh: nc.tensor.matmul(po[:, hh * 64:(hh + 1) * 64], pTc[0:127, hh, :], vc[0:127, g * 64:(g + 1) * 64], start=True, stop=True),
                     reads=[d_pTc, d_vcl], writes=[d_poc])
            pim = bk5[:, 192:224]
            for hh in range(4):
                P.op("pe", lambda hh=hh: nc.tensor.matmul(pim, pTc[0:127, hh, :], ovlb[0:127, :], start=(hh == 0), stop=(hh == 3)), reads=[d_pTc, d_cst], writes=[d_pim])
            P.op("dve", lambda: nc.vector.tensor_tensor(out=oacc[:, g * 256:(g + 1) * 256].rearrange("p (h d) -> p h d", h=4), in0=po.rearrange("p (h d) -> p h d", h=4),
                                                        in1=gts3[:, g * 4:(g + 1) * 4, 0:1].to_broadcast([128, 4, 64]), op=ALU.mult),
                 reads=[d_poc, d_gts], writes=d_oacc[g * 4:(g + 1) * 4])
            P.op("dve", lambda: nc.vector.tensor_tensor(out=score, in0=pim, in1=selmul[:, qt, :], op=ALU.mult), reads=[d_pim, d_cst], writes=[d_score])
            P.op("dve", lambda: nc.vector.tensor_tensor(out=score, in0=score, in1=seladd[:, qt, :], op=ALU.add), reads=[d_score, d_cst], writes=[d_score])
            P.op("dve", lambda: nc.vector.max(out=top8, in_=score), reads=[d_score], writes=[d_score])
            P.op("dve", lambda: nc.vector.tensor_scalar(out=selb, in0=score, scalar1=top8[:, 7:8], scalar2=NEG, op0=ALU.is_lt, op1=ALU.mult), reads=[d_score], writes=[d_selb])
        return [stA, stB, stC]

    def att_item(q2, sub, g, hh, br):
        qt = q2 * 2 + sub
        t0 = qt * 128
        ts = slice(sub * 128, (sub + 1) * 128)
        gp = g // 2
        base = (g % 2) * 64
        h = g * 4 + hh
        cp = gp * 4 + hh
        k0 = 0 if br == 1 else max(0, t0 - 256)
        nkeys = t0 + 128 - k0
        it = cnt["it"]; cnt["it"] += 1
        sm, dsm = SM[it % NBUF], d_SM[it % NBUF]
        pb_, dpb_ = pbf[it % NBUF], d_pbf[it % NBUF]
        pTt, dpTt = pT[it % NBUF], d_pT[it % NBUF]
        stt = st1[it % 4]; dmx = d_mx1[it % 4]; dss = d_ss1[it % 4]; dgs = d_gs1[it % 4]
        selb, d_selb = selb_[g % 2], d_selb_[g % 2]
        gts, d_gts = gts_[sub], d_gts_[sub]
        oacc, d_oacc = oacc_[sub], d_oacc_[sub]
        nch = nkeys // 128
        pslot = it % 3

        def stA():
            if br == 1:
                nk5 = (nkeys + 511) // 512
                for kb in range(nk5):
                    n = min(512, nkeys - kb * 512)
                    P.op("pe", lambda kb=kb, n=n: nc.tensor.matmul(K.psS[:, kb * 512:kb * 512 + n], qr[base:base + 64, cp, ts], kT[base:base + 64, 0, gp, kb * 512:kb * 512 + n], start=True, stop=True),
                         reads=[d_qr, d_kTl], writes=[K.dbank[kb]])
                nblk = nkeys // 64
                P.op("dve", lambda: nc.vector.tensor_tensor(out=sm[:, 0:nkeys].rearrange("p (b k) -> p b k", k=64), in0=K.psS[:, 0:nkeys].rearrange("p (b k) -> p b k", k=64),
                                                            in1=selb[:, 0:nblk].unsqueeze(2).to_broadcast([128, nblk, 64]), op=ALU.add),
                     reads=list(K.dbank[0:nk5]) + [d_selb], writes=[dsm])
                P.op("dve", lambda: nc.vector.tensor_tensor(out=sm[:, t0:t0 + 128], in0=sm[:, t0:t0 + 128], in1=causal, op=ALU.add), reads=[dsm, d_cst], writes=[dsm])
            else:
                pw = bk4[:, 0:nkeys]
                P.op("pe", lambda: nc.tensor.matmul(pw, qr[base:base + 64, cp, ts], kT[base:base + 64, 1, gp, k0:k0 + nkeys], start=True, stop=True),
                     reads=[d_qr, d_kTl], writes=[dbk4])
                P.op("dve", lambda: nc.vector.tensor_tensor(out=sm[:, 0:nkeys], in0=pw, in1=winmask[:, 384 - nkeys:384], op=ALU.add), reads=[dbk4, d_cst], writes=[dsm])
            P.op("dve", lambda: nc.vector.reduce_max(out=stt[:, 0:1], in_=sm[:, 0:nkeys], axis=AX.X), reads=[dsm], writes=[dmx])
            P.op("dve", lambda: nc.vector.tensor_scalar(out=stt[:, 1:2], in0=stt[:, 0:1], scalar1=-1.0, scalar2=None, op0=ALU.mult), reads=[dmx], writes=[dmx])

        def stB():
            P.op("act", lambda: nc.scalar.activation(out=pb_[:, 0:nkeys], in_=sm[:, 0:nkeys], func=AF.Exp, bias=stt[:, 1:2], scale=1.0, accum_out=stt[:, 2:3]),
                 reads=[dsm, dmx], writes=[dpb_, dss])
            for c0 in range(0, nch, 8):
                n8 = min(8, nch - c0)
                pt_, dpt_ = nT()
                for kk in range(n8):
                    P.op("pe", lambda pt_=pt_, kk=kk, c0=c0: nc.tensor.transpose(pt_[:, kk * 128:(kk + 1) * 128], pb_[:, (c0 + kk) * 128:(c0 + kk + 1) * 128], identb), reads=[dpb_, d_cst], writes=[dpt_])
                P.op("act", lambda pt_=pt_, c0=c0, n8=n8: nc.scalar.copy(out=pTt[:, c0:c0 + n8, :], in_=pt_[:, 0:n8 * 128].rearrange("p (c t) -> p c t", c=n8)), reads=[dpt_], writes=[dpTt])

        def stC():
            po = bk5[:, pslot * 64:(pslot + 1) * 64]
            dpo = d_po[pslot]
            kch0 = k0 // 128
            vi = 0 if br == 1 else 1
            for kk in range(nch):
                P.op("pe", lambda kk=kk: nc.tensor.matmul(po, pTt[:, kk, :], v[:, vi, kch0 + kk, g * 64:(g + 1) * 64], start=(kk == 0), stop=(kk == nch - 1)),
                     reads=[dpTt, d_vl], writes=[dpo])
            P.op("dve", lambda: nc.vector.reciprocal(out=stt[:, 3:4], in_=stt[:, 2:3]), reads=[dss], writes=[dgs])
            P.op("dve", lambda: nc.vector.tensor_tensor(out=stt[:, 3:4], in0=stt[:, 3:4], in1=gts[:, h * 3 + br:h * 3 + br + 1], op=ALU.mult), reads=[dgs, d_gts], writes=[dgs])
            P.op("dve", lambda: nc.vector.scalar_tensor_tensor(out=oacc[:, h * 64:(h + 1) * 64], in0=po, scalar=stt[:, 3:4], in1=oacc[:, h * 64:(h + 1) * 64], op0=ALU.mult, op1=ALU.add),
                 reads=[dpo, dgs, d_oacc[h]], writes=[d_oacc[h]])
        return [stA, stB, stC]

    for s in range(int(os.environ.get("NSA_DBG_NS", NS))):
        P.dma("sp", lambda s=s: nc.sync.dma_start(out=kT, in_=K.kvscr["kT"][s].rearrange("ti gp p k -> p ti gp k")), sem("kvl", 0, 4), writes=[d_kTl])
        P.dma("sp", lambda s=s: nc.sync.dma_start(out=v, in_=K.kvscr["v"][s].rearrange("ti p c n -> p ti c n")), sem("kvl", 1, 4), writes=[d_vl])
        P.dma("sp", lambda s=s: nc.sync.dma_start(out=kc, in_=K.kvscr["kc"][s].rearrange("gp p c -> p gp c")), sem("kvl", 2, 4), writes=[d_kcl])
        P.dma("sp", lambda s=s: nc.sync.dma_start(out=vc, in_=K.kvscr["vc"][s]), sem("kvl", 3, 4), writes=[d_vcl])
        entries = []
        for q2 in range(int(os.environ.get("NSA_DBG_Q2", 8))):
            for sub in range(2):
                order = [("c", 0)] + [("w", 0, hh) for hh in range(4)]
                for g in range(1, 4):
                    order += [("c", g)] + [("s", g - 1, hh) for hh in range(4)] + [("w", g, hh) for hh in range(4)]
                order += [("s", 3, hh) for hh in range(4)]
                for oi, od in enumerate(order):
                    pre = []
                    post = []
                    if oi == 0:
                        if sub == 0:
                            pre.append(lambda s=s, q2=q2: batch_pre(s, q2))
                        pre.append(lambda s=s, q2=q2, sub=sub: tile_pre(s, q2, sub))
                    if oi == len(order) - 1:
                        post.append(lambda s=s, q2=q2, sub=sub: tile_post(s, q2, sub))
                        if sub == 1:
                            post.append(lambda s=s, q2=q2: batch_post(s, q2))
                    if od[0] == "c":
                        mk = (lambda q2=q2, sub=sub, g=od[1]: cmp_item(q2, sub, g))
                    else:
                        mk = (lambda q2=q2, sub=sub, g=od[1], hh=od[2], br=(1 if od[0] == "s" else 2): att_item(q2, sub, g, hh, br))
                    entries.append((pre, mk, post))
        n = len(entries)
        stages = [None] * n
        for t in range(n + 2):
            if t < n:
                for f in entries[t][0]:
                    f()
                stages[t] = entries[t][1]()
                stages[t][0]()
            if 0 <= t - 1 < n:
                stages[t - 1][1]()
            if 0 <= t - 2 < n:
                stages[t - 2][2]()
                for f in entries[t - 2][2]:
                    f()
                stages[t - 2] = None

_NC_CACHE = {}


def kernel(**inputs):
    stop = os.environ.get("KSTOP", "all")
    if stop not in _NC_CACHE:
        _NC_CACHE[stop] = build_program(stop)
    nc = _NC_CACHE[stop]
    x = np.asarray(inputs["x"], np.float32)
    sp = _pack_small(inputs)
    wmaps = {k: np.ascontiguousarray(np.asarray(inputs[k], np.float32)) for k in WSHAPES}
    in_maps = []
    for c in range(8):
        xs = x[2 * c:2 * c + 2].reshape(T, D)
        m = {"xT": np.ascontiguousarray(xs.T), "spk": sp, "cst": CST_NP, "cs_tab": CS_TAB}
        m.update(wmaps)
        in_maps.append(m)
    res = run_bass_kernel_spmd(nc, in_maps, core_ids=list(range(8)))
    out = np.empty((16, S, D), np.float32)
    for c in range(8):
        o = res.results[c]["outT"]
        out[2 * c:2 * c + 2] = o.T.reshape(2, S, D)
    return out
```

```python
import numpy as np
import concourse.bass as bass
import concourse.mybir as mybir

F32 = mybir.dt.float32
BF16 = mybir.dt.bfloat16
AF = mybir.ActivationFunctionType
ALU = mybir.AluOpType
AX = mybir.AxisListType

COMPUTE = ("pe", "act", "dve", "pool")
QUEUES = ("pe", "act", "dve", "pool", "sp")
SEM_CAP = 30000
import os as _os
STRICT_SAME = _os.environ.get('BASS_STRICT_SAME', '1') == '1'


class Dep:
    __slots__ = ("w", "r", "name")

    def __init__(self, name=""):
        self.w = None
        self.r = []
        self.name = name


class Op:
    __slots__ = ("q", "fn", "waits", "marked", "midx", "dma_sem", "dma_val", "idx")

    def __init__(self, q, fn):
        self.q = q
        self.fn = fn
        self.waits = []
        self.marked = False
        self.midx = None
        self.dma_sem = None
        self.dma_val = None


class Prog:
    def __init__(self, nc):
        self.nc = nc
        self.ops = {q: [] for q in QUEUES}
        self.dma_sems = []
        self.n_dma_sems = 0

    def new_dma_sem(self):
        self.dma_sems.append(0)
        return len(self.dma_sems) - 1

    def _collect(self, q, reads, writes):
        toks = []
        for d in reads:
            if d.w is not None:
                toks.append(d.w)
        for d in writes:
            if d.w is not None:
                toks.append(d.w)
            toks.extend(d.r)
        out = []
        seen = set()
        for t in toks:
            if t[0] == "c":
                o = t[1]
                if o.q == q:
                    if q in ("pe", "sp"):
                        continue
                    if not STRICT_SAME:
                        israw = any(d.w is t for d in reads)
                        if not israw:
                            continue
                key = ("c", id(o))
            else:
                key = t
            if key in seen:
                continue
            seen.add(key)
            out.append(t)
        return out

    def op(self, q, fn, reads=(), writes=()):
        o = Op(q, fn)
        o.waits = self._collect(q, reads, writes)
        for t in o.waits:
            if t[0] == "c":
                t[1].marked = True
        tok = ("c", o)
        for d in reads:
            d.r.append(tok)
        for d in writes:
            d.w = tok
            d.r = []
        self.ops[q].append(o)
        return o

    def dma(self, q, fn, sem, reads=(), writes=()):
        kind = "sw" if q == "pool" else "hw"
        if not hasattr(self, "_semkind"):
            self._semkind = {}
        key = (sem, kind)
        if key not in self._semkind:
            if any(k[0] == sem for k in self._semkind):
                self._semkind[key] = self.new_dma_sem()
            else:
                self._semkind[key] = sem
        sem = self._semkind[key]
        o = Op(q, fn)
        o.waits = self._collect(None, reads, writes)
        for t in o.waits:
            if t[0] == "c":
                t[1].marked = True
        self.dma_sems[sem] += 16
        o.dma_sem = sem
        o.dma_val = self.dma_sems[sem]
        tok = ("d", sem, o.dma_val)
        for d in reads:
            d.r.append(tok)
        for d in writes:
            d.w = tok
            d.r = []
        self.ops[q].append(o)
        return o

    def barrier(self):
        toks = []
        for q in COMPUTE:
            if self.ops[q]:
                for o in reversed(self.ops[q]):
                    if o.fn is not None and o.dma_sem is None:
                        toks.append(("c", o))
                        o.marked = True
                        break
        for s, v in enumerate(self.dma_sems):
            if v > 0:
                toks.append(("d", s, v))
        for q in QUEUES:
            o = Op(q, None)
            o.waits = [t for t in toks if not (t[0] == "c" and t[1].q == q)]
            self.ops[q].append(o)

    def replay(self, final_deps=()):
        nc = self.nc
        import contextlib
        nmark = {}
        for q in COMPUTE:
            m = 0
            for o in self.ops[q]:
                if o.marked:
                    o.midx = m
                    m += 1
            nmark[q] = m
        final_toks = []
        for d in final_deps:
            if d.w is not None:
                final_toks.append(d.w)
        with contextlib.ExitStack() as es:
            csems = {}
            for q in COMPUTE:
                n = max(1, (nmark[q] + SEM_CAP - 1) // SEM_CAP)
                csems[q] = [es.enter_context(nc.semaphore(f"c_{q}_{i}")) for i in range(n)]
            dsems = [es.enter_context(nc.semaphore(f"d_{i}")) for i in range(len(self.dma_sems))]
            block = es.enter_context(nc.Block())
            engs = {"pe": nc.tensor, "act": nc.scalar, "dve": nc.vector, "pool": nc.gpsimd, "sp": nc.sync}

            def emit_queue(q, eng):
                seen_c = {x: -1 for x in COMPUTE}
                seen_d = {}
                def do_wait(t):
                    if t[0] == "c":
                        o = t[1]
                        if o.midx <= seen_c[o.q]:
                            return
                        seen_c[o.q] = o.midx
                        eng.wait_ge(csems[o.q][o.midx // SEM_CAP], o.midx % SEM_CAP + 1)
                    else:
                        _, s, v = t
                        if seen_d.get(s, 0) >= v:
                            return
                        seen_d[s] = v
                        eng.wait_ge(dsems[s], v)
                for o in self.ops[q]:
                    for t in o.waits:
                        do_wait(t)
                    if o.fn is None:
                        continue
                    ins = o.fn()
                    if o.dma_sem is not None:
                        ins.then_inc(dsems[o.dma_sem], 16)
                    elif o.marked:
                        ins.then_inc(csems[q][o.midx // SEM_CAP], 1)
                if q == "sp":
                    for t in final_toks:
                        do_wait(t)

            @block.tensor
            def _(e):
                emit_queue("pe", e)

            @block.scalar
            def _(e):
                emit_queue("act", e)

            @block.vector
            def _(e):
                emit_queue("dve", e)

            @block.gpsimd
            def _(e):
                emit_queue("pool", e)

            @block.sync
            def _(e):
                emit_queue("sp", e)
        return {q: len(self.ops[q]) for q in QUEUES}

import os
from concourse.bass_utils import run_bass_kernel_spmd
import contextlib
import numpy as np

S = 2048
NS = 2
T = NS * S
D = 1024
ALPHA = float(8.0 ** 0.25)
EPS = 1e-5
DFF = 2816
MFF = 3584
NEG = -1e30

SP_LNG = 0
SP_LNB = 64
SP_CW = 128
SP_CB = 192
SP_BA = 208
SP_BI = 224
SP_LAM = 240


SP_PE = 256
NSP = 320

WSHAPES = {
    "lru_w_in": (2, 1024, 2048), "lru_w_a": (2, 8, 128, 128), "lru_w_i": (2, 8, 128, 128), "lru_w_out": (2, 1024, 1024),
    "nsa_w_kv": (1024, 1536), "nsa_cmp_w1": (2, 2048, 256), "nsa_cmp_w2": (2, 256, 64),
    "nsa_w_qg": (2, 1024, 1072), "nsa_w_o": (2, 1024, 1024),
    "ffn_w_gu": (2, 1024, 5632), "ffn_w_down": (2, 2816, 1024),
    "moe_w_router": (2, 1024, 8), "moe_w_gu": (2, 8, 1024, 7168), "moe_w_down": (2, 8, 3584, 1024),
}


def _make_consts():
    cols = {}
    parts = []
    off = 0

    def add(name, arr):
        nonlocal off
        arr = np.asarray(arr, np.float32)
        assert arr.shape[0] == 128
        cols[name] = (off, off + arr.shape[1])
        parts.append(arr)
        off += arr.shape[1]

    add("ones", np.full((128, 128), 1.0 / 1024.0))
    add("ident", np.eye(128))
    add("eps", np.full((128, 1), EPS))
    add("one", np.ones((128, 1)))
    add("zero", np.zeros((128, 1)))
    sel = np.zeros((128, 8, 128), np.float32)
    for e in range(8):
        sel[e, e, :] = 1.0
    add("sel", sel.reshape(128, 1024))
    RT = np.zeros((128, 128), np.float32)
    for hb_ in range(2):
        o = hb_ * 64
        for i in range(8):
            RT[o + i + 8, o + i] = -1.0
            RT[o + i, o + i + 8] = 1.0
    add("RT", RT)
    ii = np.arange(128)[:, None]
    jj = np.arange(128)[None, :]
    add("causal", np.where(jj <= ii, 0.0, NEG))
    jw = np.arange(384)[None, :]
    rel = ii + 256 - jw
    add("winmask", np.where((rel >= 0) & (rel < 256), 0.0, NEG))
    cm = np.full((128, 16, 128), NEG, np.float32)
    for qt in range(16):
        t = qt * 128 + np.arange(128)[:, None]
        c = np.arange(127)[None, :]
        cm[:, qt, 0:127] = np.where(c * 16 + 31 <= t, 0.0, NEG)
    add("cmpmask", cm.reshape(128, 2048))
    av = np.zeros((128, 16), np.float32)
    for qt in range(16):
        av[:, qt] = (qt * 128 + np.arange(128) >= 31).astype(np.float32)
    add("anyvalid", av)
    sm_ = np.zeros((128, 16, 32), np.float32)
    sa_ = np.zeros((128, 16, 32), np.float32)
    for qt in range(16):
        t = qt * 128 + np.arange(128)[:, None]
        n = np.arange(32)[None, :]
        cur = t // 64
        forced = (n == 0) | (n == cur) | (n == cur - 1)
        causal_ = (n * 64) <= t
        sm_[:, qt, :] = np.where(forced, 0.0, np.where(causal_, 1.0, 0.0))
        sa_[:, qt, :] = np.where(forced, 1e30, np.where(causal_, 0.0, NEG))
    add("selmul", sm_.reshape(128, 512))
    add("seladd", sa_.reshape(128, 512))
    ov = np.zeros((128, 32), np.float32)
    for c in range(127):
        for n in range(32):
            if (c * 16 < n * 64 + 64) and (c * 16 + 32 > n * 64):
                ov[c, n] = 1.0
    add("ovl", ov)
    if off % 2:
        add("pad", np.zeros((128, 1)))
    return np.concatenate(parts, axis=1), cols


CST_NP, CST_COLS = _make_consts()
NCST = CST_NP.shape[1]


def _make_cs_tab():
    half = 8
    inv = 500000.0 ** (-np.arange(half, dtype=np.float32) / half)
    pos = np.arange(S, dtype=np.float32)
    ang = pos[None, :] * inv[:, None]
    cos = np.ones((64, S), np.float32)
    sin = np.zeros((64, S), np.float32)
    cos[0:8] = np.cos(ang); cos[8:16] = np.cos(ang)
    sin[0:8] = np.sin(ang); sin[8:16] = np.sin(ang)
    tab = np.stack([np.concatenate([cos, cos], 0), np.concatenate([sin, sin], 0)], 0)
    return np.ascontiguousarray(tab.astype(np.float32))


CS_TAB = _make_cs_tab()


def _pack_small(inp):
    sp = np.zeros((128, NSP), np.float32)
    f = lambda a: np.asarray(a, np.float32)
    sp[:, SP_LNG:SP_LNG + 64] = f(inp["ln_g"]).reshape(4, 2, 8, 128).transpose(3, 0, 1, 2).reshape(128, 64)
    sp[:, SP_LNB:SP_LNB + 64] = f(inp["ln_b"]).reshape(4, 2, 8, 128).transpose(3, 0, 1, 2).reshape(128, 64)
    sp[:, SP_CW:SP_CW + 64] = f(inp["lru_conv_w"]).reshape(2, 4, 8, 128).transpose(3, 0, 2, 1).reshape(128, 64)
    sp[:, SP_CB:SP_CB + 16] = f(inp["lru_conv_b"]).reshape(2, 8, 128).transpose(2, 0, 1).reshape(128, 16)
    sp[:, SP_BA:SP_BA + 16] = f(inp["lru_b_a"]).transpose(2, 0, 1).reshape(128, 16)
    sp[:, SP_BI:SP_BI + 16] = f(inp["lru_b_i"]).transpose(2, 0, 1).reshape(128, 16)
    sp[:, SP_LAM:SP_LAM + 16] = f(inp["lru_lambda"]).reshape(2, 8, 128).transpose(2, 0, 1).reshape(128, 16)
    pe = f(inp["nsa_cmp_pos"]).transpose(2, 0, 1).reshape(64, 64)
    sp[0:64, SP_PE:SP_PE + 64] = pe
    sp[64:128, SP_PE:SP_PE + 64] = pe
    return sp


class Arena:
    def __init__(self, ap, n32):
        self.ap = ap
        self.n = n32
        self.off = 0
        self.marks = []

    def reset(self):
        self.off = 0
        self.marks = []

    def mark(self):
        self.marks.append(self.off)

    def release(self):
        self.off = self.marks.pop()

    def _take(self, n32):
        assert self.off + n32 <= self.n, f"arena overflow {self.off}+{n32}>{self.n}"
        a = self.ap[:, self.off:self.off + n32]
        self.off += n32
        return a

    def f32(self, shape):
        n = int(np.prod(shape[1:]))
        a = self._take(n)
        return self._shape(a, shape)

    def bf(self, shape):
        n = int(np.prod(shape[1:]))
        n32 = (n + 1) // 2
        a = self._take(n32).bitcast(BF16)
        if n32 * 2 != n:
            a = a[:, 0:n]
        return self._shape(a, shape)

    @staticmethod
    def _shape(a, shape):
        if len(shape) == 2:
            return a
        if len(shape) == 3:
            return a.rearrange("p (a b) -> p a b", a=shape[1])
        if len(shape) == 4:
            return a.rearrange("p (a b c) -> p a b c", a=shape[1], b=shape[2])
        raise ValueError


class Ctx:
    pass


def build_program(stop="all", dbg=False):
    nc = bass.Bass("TRN2", target_bir_lowering=False)
    P = Prog(nc)
    K = Ctx()
    K.nc = nc
    K.P = P

    def din(name, shape, dt=F32):
        return nc.dram_tensor(name, list(shape), dt, kind="ExternalInput").ap()

    xT = din("xT", [D, T])
    spk = din("spk", [128, NSP])
    cst = din("cst", [128, NCST])
    cs_tab = din("cs_tab", [2, 128, S])
    K.cs_tab = cs_tab
    W = {}
    for name, shape in WSHAPES.items():
        W[name] = din(name, shape)
    outT = nc.dram_tensor("outT", [D, T], F32, kind="ExternalOutput").ap()
    hres = nc.dram_tensor("hres", [D, T], F32, kind="Internal").ap()
    kvscr = {}
    kvscr["kT"] = nc.dram_tensor("kT_scr", [NS, 2, 2, 128, S], BF16, kind="Internal").ap()
    kvscr["v"] = nc.dram_tensor("v_scr", [NS, 2, 128, 16, 256], BF16, kind="Internal").ap()
    kvscr["kc"] = nc.dram_tensor("kc_scr", [NS, 2, 128, 128], BF16, kind="Internal").ap()
    kvscr["vc"] = nc.dram_tensor("vc_scr", [NS, 128, 256], BF16, kind="Internal").ap()

    es = contextlib.ExitStack()
    with es:
        NA = 49152
        arena_t = es.enter_context(nc.sbuf_tensor("arena", [128, NA], F32))
        NPERS = NSP + NCST + 64 + 16
        A = Arena(arena_t[:], NA - NPERS)
        spk_t = arena_t[:, NA - NSP:NA]
        cst_t = arena_t[:, NA - NSP - NCST:NA - NSP]
        identb = arena_t[:, NA - NPERS:NA - NPERS + 64].bitcast(BF16)
        ovlb = arena_t[:, NA - NPERS + 64:NA - NPERS + 80].bitcast(BF16)
        K.identb = identb
        K.ovlb = ovlb
        K.final = []
        psS = es.enter_context(nc.psum_tensor("psS", [128, 2048], F32))
        psA = es.enter_context(nc.psum_tensor("psA", [128, 512], F32))
        psB = es.enter_context(nc.psum_tensor("psB", [128, 512], F32))
        psT = [es.enter_context(nc.psum_tensor(f"psT{i}", [128, 1024], BF16)) for i in range(2)]
        K.bank = [psS[:, i * 512:(i + 1) * 512] for i in range(4)] + [psA[:], psB[:]]
        K.dbank = [Dep(f"bank{i}") for i in range(6)]
        K.psS = psS
        K.psT = [psT[0][:], psT[1][:]]
        K.dpsT = [Dep("psT0"), Dep("psT1")]

        sem_pool = {}

        def sem(role, i=0, n=1):
            key = (role, i % n)
            if key not in sem_pool:
                sem_pool[key] = P.new_dma_sem()
            return sem_pool[key]

        d_spk = Dep("spk")
        d_cst = Dep("cst")
        P.dma("sp", lambda: nc.sync.dma_start(out=spk_t, in_=spk[:, :]), sem("spk"), writes=[d_spk])
        P.dma("sp", lambda: nc.sync.dma_start(out=cst_t, in_=cst[:, :]), sem("cst"), writes=[d_cst])
        _ia, _ib = CST_COLS["ident"]
        _oa, _ob = CST_COLS["ovl"]
        P.op("act", lambda: nc.scalar.copy(out=identb, in_=cst_t[:, _ia:_ib]), reads=[d_cst], writes=[Dep()])
        P.op("act", lambda: nc.scalar.copy(out=ovlb, in_=cst_t[:, _oa:_ob]), reads=[d_cst], writes=[Dep()])
        P.barrier()

        def spc(col, n=1):
            return spk_t[:, col:col + n]

        def cc(name):
            a, b = CST_COLS[name]
            return cst_t[:, a:b]

        ones_f = cc("ones")
        ident_f = cc("ident")
        eps_c = cc("eps")
        one_c = cc("one")
        zero_c = cc("zero")

        K.A = A

        def load_h_bf16(dst, src, t0, nt, dep, role, i=0):
            srcap = src.rearrange("(c p) t -> p c t", p=128)[:, :, t0:t0 + nt]
            P.dma("pool", lambda: nc.gpsimd.dma_start(out=dst, in_=srcap), sem(role, i, 2), writes=[dep])

        def postnorm_tile(src, dst, t0, nt, yfn, l, i, bufs, ddst_extra=None):
            R, dR, O, dO, SQ, dSQ, MS, dMS = bufs
            gcol = SP_LNG + (l * 2 + i) * 8
            bcol = SP_LNB + (l * 2 + i) * 8
            srcap = src.rearrange("(c p) t -> p c t", p=128)[:, :, t0:t0 + nt]
            dstap = dst.rearrange("(c p) t -> p c t", p=128)[:, :, t0:t0 + nt]
            P.dma("sp", lambda: nc.sync.dma_start(out=R[:, :, 0:nt], in_=srcap), sem("pnR"), writes=[dR])
            pm, dpm = K.bank[0][:, 0:nt], K.dbank[0]
            pq, dpq = K.bank[1][:, 0:nt], K.dbank[1]
            for c in range(8):
                yap, ydeps = yfn(c)
                Rc = R[:, c, 0:nt]
                P.op("dve", lambda Rc=Rc, yap=yap: nc.vector.scalar_tensor_tensor(out=Rc, in0=Rc, scalar=ALPHA, in1=yap, op0=ALU.mult, op1=ALU.add),
                     reads=[dR] + ydeps, writes=[dR])
                sq = SQ[c % 2][:, 0:nt]
                dsq = dSQ[c % 2]
                P.op("act", lambda Rc=Rc, sq=sq: nc.scalar.activation(out=sq, in_=Rc, func=AF.Square), reads=[dR], writes=[dsq])
                P.op("pe", lambda Rc=Rc, c=c: nc.tensor.matmul(pm, ones_f, Rc, start=(c == 0), stop=(c == 7)), reads=[dR], writes=[dpm])
                P.op("pe", lambda sq=sq, c=c: nc.tensor.matmul(pq, ones_f, sq, start=(c == 0), stop=(c == 7)), reads=[dsq], writes=[dpq])
            mean = MS[:, 0, 0:nt]
            rstd = MS[:, 1, 0:nt]
            P.op("act", lambda: nc.scalar.copy(out=mean, in_=pm), reads=[dpm], writes=[dMS])
            P.op("dve", lambda: nc.vector.tensor_tensor(out=rstd, in0=mean, in1=mean, op=ALU.mult), reads=[dMS], writes=[dMS])
            P.op("dve", lambda: nc.vector.tensor_tensor(out=rstd, in0=pq, in1=rstd, op=ALU.subtract), reads=[dMS, dpq], writes=[dMS])
            P.op("act", lambda: nc.scalar.activation(out=rstd, in_=rstd, func=AF.Sqrt, bias=eps_c, scale=1.0), reads=[dMS], writes=[dMS])
            P.op("dve", lambda: nc.vector.reciprocal(out=rstd, in_=rstd), reads=[dMS], writes=[dMS])
            for c in range(8):
                Rc = R[:, c, 0:nt]
                Oc = O[:, c, 0:nt]
                P.op("dve", lambda Rc=Rc: nc.vector.tensor_tensor(out=Rc, in0=Rc, in1=mean, op=ALU.subtract), reads=[dR, dMS], writes=[dR])
                P.op("dve", lambda Rc=Rc: nc.vector.tensor_tensor(out=Rc, in0=Rc, in1=rstd, op=ALU.mult), reads=[dR, dMS], writes=[dR])
                P.op("act", lambda Rc=Rc, Oc=Oc, c=c: nc.scalar.activation(out=Oc, in_=Rc, func=AF.Identity, bias=spc(bcol + c), scale=spc(gcol + c)),
                     reads=[dR, d_spk], writes=[dO])
            dd = Dep("dst")
            P.dma("sp", lambda: nc.sync.dma_start(out=dstap, in_=O[:, :, 0:nt]), sem("pnO"), reads=[dO], writes=[dd])
            if dst is outT:
                K.final.append(dd)
            return dd

        def alloc_pn(nt):
            R = A.f32([128, 8, nt]); O = A.f32([128, 8, nt])
            SQ = [A.f32([128, nt]) for _ in range(2)]
            MS = A.f32([128, 2, nt])
            return (R, Dep("R"), O, Dep("O"), SQ, [Dep("sq0"), Dep("sq1")], MS, Dep("MS"))

        def lru_phase(l, src, dst):
            P.barrier()
            A.reset()
            wout = A.bf([128, 8, 1024]); d_wout = Dep()
            P.dma("pool", lambda: nc.gpsimd.dma_start(out=wout, in_=W["lru_w_out"][l].rearrange("(j p) n -> p j n", p=128)), sem("w0"), writes=[d_wout])
            wa = A.bf([128, 8, 128]); wi = A.bf([128, 8, 128]); d_wa = Dep(); d_wi = Dep()
            P.dma("pool", lambda: nc.gpsimd.dma_start(out=wa, in_=W["lru_w_a"][l].rearrange("j c d -> c j d")), sem("w1"), writes=[d_wa])
            P.dma("pool", lambda: nc.gpsimd.dma_start(out=wi, in_=W["lru_w_i"][l].rearrange("j c d -> c j d")), sem("w2"), writes=[d_wi])
            hb = A.bf([128, 8, S]); d_hb = Dep()
            ysb = A.bf([128, 8, S]); d_ys = [Dep() for _ in range(8)]
            wgx = [A.bf([128, 8, 2, 128]) for _ in range(2)]; d_wgx = [Dep(), Dep()]
            sp8 = A.f32([128, 8]); d_sp8 = Dep()
            lam = spc(SP_LAM + l * 8, 8)
            P.op("act", lambda: nc.scalar.activation(out=sp8, in_=lam, func=AF.Exp, scale=-1.0), reads=[d_spk], writes=[d_sp8])
            P.op("act", lambda: nc.scalar.activation(out=sp8, in_=sp8, func=AF.Ln, bias=one_c, scale=1.0), reads=[d_sp8], writes=[d_sp8])
            P.op("dve", lambda: nc.vector.tensor_scalar(out=sp8, in0=sp8, scalar1=-8.0, scalar2=None, op0=ALU.mult), reads=[d_sp8], writes=[d_sp8])
            w_in_v = W["lru_w_in"][l].rearrange("(kc p) (two n) -> p kc two n", p=128, two=2)
            for s in range(NS):
                load_h_bf16(hb, src, s * S, S, d_hb, "hb")
                A.mark()
                G = A.f32([128, S]); XP = A.f32([128, S + 8]); XR = A.f32([128, S]); RA = A.f32([128, S])
                IB = A.f32([128, S]); T1 = A.f32([128, S]); H = A.f32([128, S]); xrb = A.bf([128, S])
                dG, dXP, dXR, dRA, dIB, dT1, dH, dxrb = [Dep(n) for n in "G XP XR RA IB T1 H xrb".split()]
                P.op("pool", lambda XP=XP: nc.gpsimd.memset(XP[:, 0:3], 0.0), writes=[dXP])
                bi = 0
                for j in range(8):
                    wt = wgx[j % 2]; dwt = d_wgx[j % 2]
                    P.dma("pool", lambda wt=wt, j=j: nc.gpsimd.dma_start(out=wt[:, :, 0, :], in_=w_in_v[:, :, 0, j * 128:(j + 1) * 128]), sem("wgx", j, 2), writes=[dwt])
                    P.dma("pool", lambda wt=wt, j=j: nc.gpsimd.dma_start(out=wt[:, :, 1, :], in_=w_in_v[:, :, 1, j * 128:(j + 1) * 128]), sem("wgx", j, 2), reads=[dwt], writes=[dwt])
                    for tt in range(4):
                        sl = slice(tt * 512, (tt + 1) * 512)
                        for which in range(2):
                            b = bi % 4; bi += 1
                            pb, dpb = K.bank[b], K.dbank[b]
                            for kc in range(8):
                                P.op("pe", lambda pb=pb, wt=wt, kc=kc, which=which, sl=sl: nc.tensor.matmul(pb, wt[:, kc, which, :], hb[:, kc, sl], start=(kc == 0), stop=(kc == 7)),
                                     reads=[dwt, d_hb], writes=[dpb])
                            if which == 0:
                                P.op("act", lambda pb=pb, sl=sl, G=G: nc.scalar.activation(out=G[:, sl], in_=pb, func=AF.Gelu_apprx_tanh), reads=[dpb], writes=[dG])
                            else:
                                P.op("dve", lambda pb=pb, tt=tt, XP=XP: nc.vector.tensor_copy(out=XP[:, 3 + tt * 512:3 + (tt + 1) * 512], in_=pb), reads=[dpb], writes=[dXP])
                    cw = SP_CW + (l * 8 + j) * 4
                    cb = SP_CB + l * 8 + j
                    P.op("dve", lambda XP=XP, XR=XR, cw=cw, cb=cb: nc.vector.tensor_scalar(out=XR, in0=XP[:, 0:S], scalar1=spc(cw), scalar2=spc(cb), op0=ALU.mult, op1=ALU.add),
                         reads=[dXP, d_spk], writes=[dXR])
                    for k in range(1, 4):
                        P.op("dve", lambda XP=XP, XR=XR, cw=cw, k=k: nc.vector.scalar_tensor_tensor(out=XR, in0=XP[:, k:k + S], scalar=spc(cw + k), in1=XR, op0=ALU.mult, op1=ALU.add),
                             reads=[dXP, dXR, d_spk], writes=[dXR])
                    P.op("act", lambda XR=XR, xrb=xrb: nc.scalar.copy(out=xrb, in_=XR), reads=[dXR], writes=[dxrb])
                    for tt in range(4):
                        sl = slice(tt * 512, (tt + 1) * 512)
                        for which in range(2):
                            b = bi % 4; bi += 1
                            pb, dpb = K.bank[b], K.dbank[b]
                            wmat, dw = (wa, d_wa) if which == 0 else (wi, d_wi)
                            bcol = (SP_BA if which == 0 else SP_BI) + l * 8 + j
                            dstt, ddst = (RA, dRA) if which == 0 else (IB, dIB)
                            P.op("pe", lambda pb=pb, wmat=wmat, j=j, sl=sl, xrb=xrb: nc.tensor.matmul(pb, wmat[:, j, :], xrb[:, sl], start=True, stop=True), reads=[dw, dxrb], writes=[dpb])
                            P.op("act", lambda pb=pb, dstt=dstt, sl=sl, bcol=bcol: nc.scalar.activation(out=dstt[:, sl], in_=pb, func=AF.Sigmoid, bias=spc(bcol), scale=1.0),
                                 reads=[dpb, d_spk], writes=[ddst])
                    P.op("act", lambda RA=RA, j=j: nc.scalar.activation(out=RA, in_=RA, func=AF.Exp, scale=sp8[:, j:j + 1]), reads=[dRA, d_sp8], writes=[dRA])
                    P.op("dve", lambda RA=RA, T1=T1: nc.vector.tensor_tensor(out=T1, in0=RA, in1=RA, op=ALU.mult), reads=[dRA], writes=[dT1])
                    P.op("dve", lambda T1=T1: nc.vector.tensor_scalar(out=T1, in0=T1, scalar1=-1.0, scalar2=1.0, op0=ALU.mult, op1=ALU.add), reads=[dT1], writes=[dT1])
                    P.op("act", lambda T1=T1: nc.scalar.activation(out=T1, in_=T1, func=AF.Sqrt, bias=zero_c, scale=1.0), reads=[dT1], writes=[dT1])
                    P.op("dve", lambda IB=IB, XR=XR: nc.vector.tensor_tensor(out=IB, in0=IB, in1=XR, op=ALU.mult), reads=[dIB, dXR], writes=[dIB])
                    P.op("dve", lambda IB=IB, T1=T1: nc.vector.tensor_tensor(out=IB, in0=IB, in1=T1, op=ALU.mult), reads=[dIB, dT1], writes=[dIB])
                    P.op("dve", lambda RA=RA, IB=IB, H=H: nc.vector.tensor_tensor_scan(out=H, data0=RA, data1=IB, initial=0.0, op0=ALU.mult, op1=ALU.add), reads=[dRA, dIB], writes=[dH])
                    P.op("dve", lambda H=H, G=G, j=j: nc.vector.tensor_tensor(out=ysb[:, j, :], in0=H, in1=G, op=ALU.mult), reads=[dH, dG], writes=[d_ys[j]])
                P.barrier()
                A.release()
                A.mark()
                bufs = alloc_pn(512)
                yb = 0
                for tt in range(4):
                    t0 = s * S + tt * 512

                    def yfn(c, tt=tt):
                        nonlocal yb
                        b = 4 + (yb % 2); yb += 1
                        pb, dpb = K.bank[b], K.dbank[b]
                        for j in range(8):
                            P.op("pe", lambda pb=pb, j=j, c=c, tt=tt: nc.tensor.matmul(pb, wout[:, j, c * 128:(c + 1) * 128], ysb[:, j, tt * 512:(tt + 1) * 512], start=(j == 0), stop=(j == 7)),
                                 reads=[d_wout, d_ys[j]], writes=[dpb])
                        return pb, [dpb]
                    postnorm_tile(src, dst, t0, 512, yfn, l, 0, bufs)
                P.barrier()
                A.release()

        def ffn_phase(l, src, dst, moe):
            P.barrier()
            A.reset()
            li = l // 2
            NT = 1024
            if moe:
                FF = MFF
                units = [(e, f0, 4) for e in range(8) for f0 in range(0, 28, 4)]
                wgu_of = lambda e: W["moe_w_gu"][li, e]
                wd_of = lambda e: W["moe_w_down"][li, e]
            else:
                FF = DFF
                units = [(0, f0, min(4, 22 - f0)) for f0 in range(0, 22, 4)]
                wgu_of = lambda e: W["ffn_w_gu"][li]
                wd_of = lambda e: W["ffn_w_down"][li]
            hb = A.bf([128, 8, NT]); d_hb = Dep()
            acc = A.f32([128, 8, NT]); d_acc = [Dep() for _ in range(2)]
            wgu_s = [A.bf([128, 8, 2, 512]) for _ in range(2)]; d_wgu = [Dep(), Dep()]
            wd_s = [A.bf([128, 4, 1024]) for _ in range(2)]; d_wd = [Dep(), Dep()]
            act = [A.bf([128, 4, 512]) for _ in range(2)]; d_act = [Dep(), Dep()]
            sg = [A.f32([128, 512]) for _ in range(2)]; d_sg = [Dep(), Dep()]
            bufs = alloc_pn(512)
            if moe:
                gb = A.f32([128, 2, 512]); d_gb = Dep()
                gatesT = A.f32([128, NT]); d_gT = Dep()
                wr = A.f32([128, 8, 8]); d_wr = Dep()
                P.dma("sp", lambda: nc.sync.dma_start(out=wr, in_=W["moe_w_router"][li].rearrange("(kc p) e -> p kc e", p=128)), sem("w0"), writes=[d_wr])
                lg = A.f32([128, 8]); mx8 = A.f32([128, 8]); nm = A.f32([128, 1]); ex = A.f32([128, 8]); msk = A.f32([128, 8]); den = A.f32([128, 1])
                d_r = Dep("router")
            ui = 0
            for st in range(T // NT):
                t0 = st * NT
                load_h_bf16(hb, src, t0, NT, d_hb, "hb")
                P.op("pool", lambda: nc.gpsimd.memset(acc.rearrange("p a b -> p (a b)"), 0.0), writes=d_acc)
                if moe:
                    R, dR = bufs[0], bufs[1]
                    for tt in range(2):
                        srcap = src.rearrange("(c p) t -> p c t", p=128)[:, :, t0 + tt * 512:t0 + (tt + 1) * 512]
                        P.dma("sp", lambda srcap=srcap: nc.sync.dma_start(out=R, in_=srcap), sem("pnR"), writes=[dR])
                        for sub in range(4):
                            pl, dpl = K.bank[4][:, 0:8], K.dbank[4]
                            for kc in range(8):
                                P.op("pe", lambda kc=kc, sub=sub: nc.tensor.matmul(pl, R[:, kc, sub * 128:(sub + 1) * 128], wr[:, kc, :], start=(kc == 0), stop=(kc == 7)),
                                     reads=[dR, d_wr], writes=[dpl])
                            P.op("dve", lambda: nc.vector.tensor_copy(out=lg, in_=pl), reads=[dpl], writes=[d_r])
                            P.op("dve", lambda: nc.vector.max(out=mx8, in_=lg), reads=[d_r], writes=[d_r])
                            P.op("dve", lambda: nc.vector.tensor_scalar(out=nm, in0=mx8[:, 0:1], scalar1=-1.0, scalar2=None, op0=ALU.mult), reads=[d_r], writes=[d_r])
                            P.op("act", lambda: nc.scalar.activation(out=ex, in_=lg, func=AF.Exp, bias=nm, scale=1.0), reads=[d_r], writes=[d_r])
                            P.op("dve", lambda: nc.vector.tensor_scalar(out=msk, in0=lg, scalar1=mx8[:, 1:2], scalar2=None, op0=ALU.is_ge), reads=[d_r], writes=[d_r])
                            P.op("dve", lambda: nc.vector.tensor_tensor(out=ex, in0=ex, in1=msk, op=ALU.mult), reads=[d_r], writes=[d_r])
                            P.op("dve", lambda: nc.vector.reduce_sum(out=den, in_=ex, axis=AX.X), reads=[d_r], writes=[d_r])
                            P.op("dve", lambda: nc.vector.reciprocal(out=den, in_=den), reads=[d_r], writes=[d_r])
                            P.op("dve", lambda: nc.vector.tensor_scalar(out=ex, in0=ex, scalar1=den, scalar2=None, op0=ALU.mult), reads=[d_r], writes=[d_r])
                            pt, dpt = K.bank[5][0:8, 0:128], K.dbank[5]
                            P.op("pe", lambda: nc.tensor.transpose(pt, ex, ident_f), reads=[d_r, d_cst], writes=[dpt])
                            c0 = tt * 512 + sub * 128
                            P.op("act", lambda c0=c0: nc.scalar.copy(out=gatesT[0:8, c0:c0 + 128], in_=pt), reads=[dpt], writes=[d_gT])
                bi = 0
                di = 0
                cur_e = -1
                for (e, f0, fcu) in units:
                    slot = ui % 2; ui += 1
                    wg_t, dwg = wgu_s[slot], d_wgu[slot]
                    wd_t, dwd = wd_s[slot], d_wd[slot]
                    wsrc = wgu_of(e).rearrange("(kc p) (two f) -> p kc two f", p=128, two=2)[:, :, :, f0 * 128:(f0 + fcu) * 128]
                    P.dma("pool", lambda wg_t=wg_t, wsrc=wsrc, fcu=fcu: nc.gpsimd.dma_start(out=wg_t[:, :, 0, 0:fcu * 128], in_=wsrc[:, :, 0, :]), sem("wgu", slot, 2), writes=[dwg])
                    P.dma("pool", lambda wg_t=wg_t, wsrc=wsrc, fcu=fcu: nc.gpsimd.dma_start(out=wg_t[:, :, 1, 0:fcu * 128], in_=wsrc[:, :, 1, :]), sem("wgu", slot, 2), reads=[dwg], writes=[dwg])
                    dsrc = wd_of(e)[f0 * 128:(f0 + fcu) * 128, :].rearrange("(fc p) n -> p fc n", p=128)
                    P.dma("pool", lambda wd_t=wd_t, dsrc=dsrc, fcu=fcu: nc.gpsimd.dma_start(out=wd_t[:, 0:fcu, :], in_=dsrc), sem("wd", slot, 2), writes=[dwd])
                    if moe and e != cur_e:
                        cur_e = e
                        for tt in range(2):
                            pb, dpb = K.bank[4], K.dbank[4]
                            P.op("pe", lambda pb=pb, e=e, tt=tt: nc.tensor.matmul(pb, cc("sel")[0:8, e * 128:(e + 1) * 128], gatesT[0:8, tt * 512:(tt + 1) * 512], start=True, stop=True),
                                 reads=[d_gT, d_cst], writes=[dpb])
                            P.op("act", lambda pb=pb, tt=tt: nc.scalar.copy(out=gb[:, tt, :], in_=pb), reads=[dpb], writes=[d_gb])
                    for tt in range(2):
                        sl = slice(tt * 512, (tt + 1) * 512)
                        at, dat = act[tt], d_act[tt]
                        for fc in range(fcu):
                            bg = bi % 2; bu = 2 + bi % 2; bi += 1
                            pg, dpg = K.bank[bg], K.dbank[bg]
                            pu, dpu = K.bank[bu], K.dbank[bu]
                            for kc in range(8):
                                P.op("pe", lambda pg=pg, wg_t=wg_t, kc=kc, fc=fc, sl=sl: nc.tensor.matmul(pg, wg_t[:, kc, 0, fc * 128:(fc + 1) * 128], hb[:, kc, sl], start=(kc == 0), stop=(kc == 7)),
                                     reads=[dwg, d_hb], writes=[dpg])
                            for kc in range(8):
                                P.op("pe", lambda pu=pu, wg_t=wg_t, kc=kc, fc=fc, sl=sl: nc.tensor.matmul(pu, wg_t[:, kc, 1, fc * 128:(fc + 1) * 128], hb[:, kc, sl], start=(kc == 0), stop=(kc == 7)),
                                     reads=[dwg, d_hb], writes=[dpu])
                            sgt, dsgt = sg[bi % 2], d_sg[bi % 2]
                            P.op("act", lambda pg=pg, sgt=sgt: nc.scalar.activation(out=sgt, in_=pg, func=AF.Silu), reads=[dpg], writes=[dsgt])
                            if moe:
                                P.op("dve", lambda sgt=sgt, tt=tt: nc.vector.tensor_tensor(out=sgt, in0=sgt, in1=gb[:, tt, :], op=ALU.mult), reads=[dsgt, d_gb], writes=[dsgt])
                            P.op("dve", lambda sgt=sgt, pu=pu, at=at, fc=fc: nc.vector.tensor_tensor(out=at[:, fc, :], in0=sgt, in1=pu, op=ALU.mult), reads=[dsgt, dpu], writes=[dat])
                    for tt in range(2):
                        sl = slice(tt * 512, (tt + 1) * 512)
                        at, dat = act[tt], d_act[tt]
                        for c in range(8):
                            b = 4 + di % 2; di += 1
                            pd, dpd = K.bank[b], K.dbank[b]
                            for fc in range(fcu):
                                P.op("pe", lambda pd=pd, wd_t=wd_t, fc=fc, c=c, at=at, fcu=fcu: nc.tensor.matmul(pd, wd_t[:, fc, c * 128:(c + 1) * 128], at[:, fc, :], start=(fc == 0), stop=(fc == fcu - 1)),
                                     reads=[dwd, dat], writes=[dpd])
                            P.op("dve", lambda pd=pd, c=c, sl=sl: nc.vector.tensor_tensor(out=acc[:, c, sl], in0=acc[:, c, sl], in1=pd, op=ALU.add), reads=[dpd, d_acc[tt]], writes=[d_acc[tt]])
                for tt in range(2):
                    def yfn(c, tt=tt):
                        return acc[:, c, tt * 512:(tt + 1) * 512], [d_acc[tt]]
                    postnorm_tile(src, dst, t0 + tt * 512, 512, yfn, l, 1, bufs)

        K.lru_phase = lru_phase
        K.ffn_phase = ffn_phase
        K.postnorm_tile = postnorm_tile
        K.alloc_pn = alloc_pn
        K.load_h_bf16 = load_h_bf16
        K.sem = sem
        K.cc = cc
        K.spc = spc
        K.d_cst = d_cst
        K.d_spk = d_spk
        K.W = W
        K.kvscr = kvscr

        seq = [("lru0", lambda d: lru_phase(0, xT, d)),
               ("ffn0", lambda d: ffn_phase(0, hres, d, False)),
               ("lru1", lambda d: lru_phase(1, hres, d)),
               ("ffn1", lambda d: ffn_phase(1, hres, d, True)),
               ("nsa2", lambda d: (kv_phase(K, hres), nsa_phase(K, 2, hres, d))),
               ("ffn2", lambda d: ffn_phase(2, hres, d, False)),
               ("nsa3", lambda d: nsa_phase(K, 3, hres, d)),
               ("all", lambda d: ffn_phase(3, hres, d, True))]
        if stop == "nsaonly":
            seq = [("nsaonly", lambda d: nsa_phase(K, 2, xT, d))]
        for name, fn in seq:
            if name == stop:
                fn(outT)
                break
            fn(hres)
        counts = P.replay(K.final)
        print("op counts", counts, "dma sems", len(P.dma_sems), "final", len(K.final))
    return nc

def kv_phase(K, src):
    nc, P, A, W = K.nc, K.P, K.A, K.W
    sem, cc, spc = K.sem, K.cc, K.spc
    P.barrier()
    A.reset()
    wkv = A.bf([128, 8, 1536]); d_wkv = Dep()
    P.dma("pool", lambda: nc.gpsimd.dma_start(out=wkv, in_=W["nsa_w_kv"].rearrange("(kc p) n -> p kc n", p=128)), sem("w0"), writes=[d_wkv])
    w1 = A.bf([128, 2, 32, 256]); d_w1 = [Dep() for _ in range(4)]
    for tkv in range(2):
        for half in range(2):
            P.dma("pool", lambda tkv=tkv, half=half: nc.gpsimd.dma_start(out=w1[half * 64:(half + 1) * 64, tkv, :, :], in_=W["nsa_cmp_w1"][tkv].rearrange("(l d) m -> d l m", d=64)),
                  sem("w1", tkv * 2 + half, 4), writes=[d_w1[tkv * 2 + half]])
    w2k = A.bf([128, 2, 128]); d_w2k = [Dep(), Dep()]
    for half in range(2):
        P.dma("pool", lambda half=half: nc.gpsimd.dma_start(out=w2k[:, :, half * 64:(half + 1) * 64], in_=W["nsa_cmp_w2"][0].rearrange("(mc p) d -> p mc d", p=128)),
              sem("w2k", half, 2), writes=[d_w2k[half]])
    w2v = A.bf([128, 2, 64]); d_w2v = Dep()
    P.dma("pool", lambda: nc.gpsimd.dma_start(out=w2v, in_=W["nsa_cmp_w2"][1].rearrange("(mc p) d -> p mc d", p=128)), sem("w2v"), writes=[d_w2v])
    peb = A.bf([128, 64]); d_peb = Dep()
    P.op("act", lambda: nc.scalar.copy(out=peb, in_=spc(SP_PE, 64)), reads=[K.d_spk], writes=[d_peb])
    biasH = A.f32([128, 4]); d_bH = Dep()
    RT = cc("RT")
    for tkv in range(2):
        for mc in range(2):
            pb, dpb = K.bank[4 + (mc % 2)][:, 0:1], K.dbank[4 + (mc % 2)]
            for l in range(32):
                P.op("pe", lambda pb=pb, tkv=tkv, mc=mc, l=l: nc.tensor.matmul(pb, w1[0:64, tkv, l, mc * 128:(mc + 1) * 128], peb[0:64, tkv * 32 + l:tkv * 32 + l + 1], start=(l == 0), stop=(l == 31)),
                     reads=[d_w1[tkv * 2], d_peb], writes=[dpb])
            P.op("act", lambda pb=pb, tkv=tkv, mc=mc: nc.scalar.copy(out=biasH[:, tkv * 2 + mc:tkv * 2 + mc + 1], in_=pb), reads=[dpb], writes=[d_bH])
    hb = A.bf([128, 8, S]); d_hb = Dep()
    cos_t = A.f32([128, S]); sin_t = A.f32([128, S]); d_cs = [Dep(), Dep()]
    P.dma("sp", lambda: nc.sync.dma_start(out=cos_t, in_=K.cs_tab[0]), sem("cs", 0, 2), writes=[d_cs[0]])
    P.dma("sp", lambda: nc.sync.dma_start(out=sin_t, in_=K.cs_tab[1]), sem("cs", 1, 2), writes=[d_cs[1]])
    kraw = [A.f32([128, 512]) for _ in range(2)]; d_kraw = [Dep(), Dep()]
    k1 = [A.f32([128, 512]) for _ in range(2)]; d_k1 = [Dep(), Dep()]
    kT_sb = [A.bf([128, S]) for _ in range(2)]; d_kT = [Dep(), Dep()]
    v_sb = A.bf([128, 16, 256]); d_v = Dep()
    csrc = A.bf([128, S]); d_csrc = Dep()
    hid = A.bf([128, 2, 128]); d_hid = Dep()
    kc_sb = A.bf([128, 128]); d_kc = Dep()
    vc_sb = A.bf([128, 256]); d_vc = Dep()
    bi = 0
    it = 0
    for s in range(NS):
        K.load_h_bf16(hb, src, s * S, S, d_hb, "hb")
        for ti, typ in enumerate((2, 4)):
            for gp in range(2):
                col0 = typ * 256 + gp * 128
                kt, dkt = kT_sb[it % 2], d_kT[it % 2]; it += 1
                for tt in range(4):
                    sl = slice(tt * 512, (tt + 1) * 512)
                    b = 4 + bi % 2; bi += 1
                    pb, dpb = K.bank[b], K.dbank[b]
                    for kc in range(8):
                        P.op("pe", lambda pb=pb, kc=kc, col0=col0, sl=sl: nc.tensor.matmul(pb, wkv[:, kc, col0:col0 + 128], hb[:, kc, sl], start=(kc == 0), stop=(kc == 7)),
                             reads=[d_wkv, d_hb], writes=[dpb])
                    kr, dkr = kraw[tt % 2], d_kraw[tt % 2]
                    kk, dkk = k1[tt % 2], d_k1[tt % 2]
                    P.op("act", lambda kr=kr, pb=pb: nc.scalar.copy(out=kr, in_=pb), reads=[dpb], writes=[dkr])
                    b2 = 4 + bi % 2; bi += 1
                    pr, dpr = K.bank[b2], K.dbank[b2]
                    P.op("pe", lambda pr=pr, kr=kr: nc.tensor.matmul(pr, RT, kr, start=True, stop=True), reads=[dkr, K.d_cst], writes=[dpr])
                    P.op("dve", lambda kk=kk, kr=kr, sl=sl: nc.vector.tensor_tensor(out=kk, in0=kr, in1=cos_t[:, sl], op=ALU.mult), reads=[dkr, d_cs[0]], writes=[dkk])
                    P.op("dve", lambda kr=kr, pr=pr, sl=sl: nc.vector.tensor_tensor(out=kr, in0=pr, in1=sin_t[:, sl], op=ALU.mult), reads=[dpr, d_cs[1]], writes=[dkr])
                    P.op("dve", lambda kt=kt, kk=kk, kr=kr, sl=sl: nc.vector.tensor_tensor(out=kt[:, sl], in0=kk, in1=kr, op=ALU.add), reads=[dkk, dkr], writes=[dkt])
                dst = K.kvscr["kT"][s, ti, gp]
                P.dma("sp", lambda kt=kt, dst=dst: nc.sync.dma_start(out=dst, in_=kt), sem("kvst", it, 2), reads=[dkt], writes=[Dep()])
        for ti, typ in enumerate((3, 5)):
            for kc16 in range(16):
                b = 4 + bi % 2; bi += 1
                pb, dpb = K.bank[b][:, 0:256], K.dbank[b]
                for kc in range(8):
                    P.op("pe", lambda pb=pb, kc=kc, kc16=kc16, typ=typ: nc.tensor.matmul(pb, hb[:, kc, kc16 * 128:(kc16 + 1) * 128], wkv[:, kc, typ * 256:(typ + 1) * 256], start=(kc == 0), stop=(kc == 7)),
                         reads=[d_wkv, d_hb], writes=[dpb])
                P.op("act", lambda pb=pb, kc16=kc16: nc.scalar.copy(out=v_sb[:, kc16, :], in_=pb), reads=[dpb], writes=[d_v])
            dst = K.kvscr["v"][s, ti]
            P.dma("sp", lambda dst=dst: nc.sync.dma_start(out=dst, in_=v_sb), sem("kvst2"), reads=[d_v], writes=[Dep()])
        for tkv in range(2):
            for gp in range(2):
                col0 = tkv * 256 + gp * 128
                for tt in range(4):
                    sl = slice(tt * 512, (tt + 1) * 512)
                    b = 4 + bi % 2; bi += 1
                    pb, dpb = K.bank[b], K.dbank[b]
                    for kc in range(8):
                        P.op("pe", lambda pb=pb, kc=kc, col0=col0, sl=sl: nc.tensor.matmul(pb, wkv[:, kc, col0:col0 + 128], hb[:, kc, sl], start=(kc == 0), stop=(kc == 7)),
                             reads=[d_wkv, d_hb], writes=[dpb])
                    P.op("act", lambda pb=pb, sl=sl: nc.scalar.copy(out=csrc[:, sl], in_=pb), reads=[dpb], writes=[d_csrc])
                for half in range(2):
                    g = gp * 2 + half
                    base = half * 64
                    for mc in range(2):
                        b = 4 + bi % 2; bi += 1
                        pb, dpb = K.bank[b][:, 0:127], K.dbank[b]
                        for l in range(32):
                            P.op("pe", lambda pb=pb, base=base, tkv=tkv, l=l, mc=mc: nc.tensor.matmul(pb, w1[base:base + 64, tkv, l, mc * 128:(mc + 1) * 128], csrc[base:base + 64, l:l + 16 * 126 + 1:16], start=(l == 0), stop=(l == 31)),
                                 reads=[d_w1[tkv * 2 + half], d_csrc], writes=[dpb])
                        P.op("act", lambda pb=pb, mc=mc, tkv=tkv: nc.scalar.activation(out=hid[:, mc, 0:127], in_=pb, func=AF.Gelu_apprx_tanh, bias=biasH[:, tkv * 2 + mc:tkv * 2 + mc + 1], scale=1.0),
                             reads=[dpb, d_bH], writes=[d_hid])
                    b = 4 + bi % 2; bi += 1
                    if tkv == 0:
                        pb, dpb = K.bank[b][:, 0:127], K.dbank[b]
                        for mc in range(2):
                            P.op("pe", lambda pb=pb, mc=mc: nc.tensor.matmul(pb, w2k[:, mc, :], hid[:, mc, 0:127], start=(mc == 0), stop=(mc == 1)), reads=d_w2k + [d_hid], writes=[dpb])
                        P.op("act", lambda pb=pb, base=base: nc.scalar.copy(out=kc_sb[base:base + 64, 0:127], in_=pb[base:base + 64, :]), reads=[dpb], writes=[d_kc])
                    else:
                        pb, dpb = K.bank[b][0:127, 0:64], K.dbank[b]
                        for mc in range(2):
                            P.op("pe", lambda pb=pb, mc=mc: nc.tensor.matmul(pb, hid[:, mc, 0:127], w2v[:, mc, :], start=(mc == 0), stop=(mc == 1)), reads=[d_w2v, d_hid], writes=[dpb])
                        P.op("act", lambda pb=pb, g=g: nc.scalar.copy(out=vc_sb[0:127, g * 64:(g + 1) * 64], in_=pb), reads=[dpb], writes=[d_vc])
                if tkv == 0:
                    dst = K.kvscr["kc"][s, gp]
                    P.dma("sp", lambda dst=dst: nc.sync.dma_start(out=dst, in_=kc_sb), sem("kvst3"), reads=[d_kc], writes=[Dep()])
            if tkv == 1:
                dst = K.kvscr["vc"][s]
                P.dma("sp", lambda dst=dst: nc.sync.dma_start(out=dst, in_=vc_sb), sem("kvst4"), reads=[d_vc], writes=[Dep()])


def nsa_phase(K, l, src, dst):
    nc, P, A, W = K.nc, K.P, K.A, K.W
    sem, cc, spc = K.sem, K.cc, K.spc
    lb = l - 2
    P.barrier()
    A.reset()
    d_cst = K.d_cst
    wq = A.bf([128, 8, 8, 128]); d_wq = []
    wq_src = W["nsa_w_qg"][lb][:, 0:1024].rearrange("(kc p) (g hh d) -> p kc g hh d", p=128, g=4, hh=4)
    for gp in range(2):
        for half in range(2):
            for hh in range(4):
                if not d_wq:
                    d_wq.append(Dep())
                P.dma("pool", lambda gp=gp, half=half, hh=hh: nc.gpsimd.dma_start(out=wq[:, :, gp * 4 + hh, half * 64:(half + 1) * 64], in_=wq_src[:, :, gp * 2 + half, hh, :]),
                      sem("w1", 0, 4), writes=[d_wq[0]])
    wgt = A.bf([128, 8, 48]); d_wgt = Dep()
    P.dma("pool", lambda: nc.gpsimd.dma_start(out=wgt, in_=W["nsa_w_qg"][lb][:, 1024:1072].rearrange("(kc p) n -> p kc n", p=128)), sem("w2v"), writes=[d_wgt])
    wo = A.bf([128, 8, 1024]); d_wo = Dep()
    P.dma("pool", lambda: nc.gpsimd.dma_start(out=wo, in_=W["nsa_w_o"][lb].rearrange("(j p) n -> p j n", p=128)), sem("w0"), writes=[d_wo])
    kT = A.bf([128, 2, 2, S]); d_kTl = Dep()
    v = A.bf([128, 2, 16, 256]); d_vl = Dep()
    kc = A.bf([128, 2, 128]); d_kcl = Dep()
    vc = A.bf([128, 256]); d_vcl = Dep()
    hb2 = [A.bf([128, 8, 256]) for _ in range(2)]; d_hb2 = [Dep(), Dep()]
    cs2 = [A.f32([128, 2, 256]) for _ in range(2)]; d_cs2 = [Dep(), Dep()]
    qraw = [A.f32([128, 256]) for _ in range(2)]; d_qraw = [Dep(), Dep()]
    qt1 = [A.f32([128, 256]) for _ in range(2)]; d_qt1 = [Dep(), Dep()]
    qb = A.bf([128, 8, 256]); d_qb = Dep()
    qr = A.bf([128, 8, 256]); d_qr = Dep()
    gts_ = [A.f32([128, 48]) for _ in range(2)]; d_gts_ = [Dep(), Dep()]
    oacc_ = [A.f32([128, 1024]) for _ in range(2)]; d_oacc_ = [[Dep() for _ in range(16)] for _ in range(2)]
    ob = A.bf([128, 1024]); d_ob = Dep()
    oT_ = [A.bf([128, 8, 256]) for _ in range(2)]; d_oT_ = [Dep(), Dep()]
    _sc = A.f32([128, 4, 128]); SC_ = [_sc, _sc]; _dsc = Dep(); d_SC_ = [_dsc, _dsc]
    _pcx = A.f32([128, 4, 128]); PCX_ = [_pcx, _pcx]; _dpcx = [Dep() for _ in range(4)]; d_PCX_ = [_dpcx, _dpcx]
    _pn = A.bf([128, 4, 128]); pn_ = [_pn, _pn]; _dpn = Dep(); d_pn_ = [_dpn, _dpn]
    _ptc = A.bf([128, 4, 128]); pTc_ = [_ptc, _ptc]; _dptc = Dep(); d_pTc_ = [_dptc, _dptc]
    st4_ = [A.f32([128, 16]) for _ in range(2)]
    d_mx4_ = [Dep(), Dep()]; d_ss4_ = [[Dep() for _ in range(4)] for _ in range(2)]; d_rs4_ = [Dep(), Dep()]
    score_ = [A.f32([128, 32]) for _ in range(2)]; top8_ = [A.f32([128, 8]) for _ in range(2)]; selb_ = [A.f32([128, 32]) for _ in range(2)]
    d_score_ = [Dep(), Dep()]; d_selb_ = [Dep(), Dep()]
    NBUF = 2
    SM = [A.f32([128, S]) for _ in range(NBUF)]; d_SM = [Dep() for _ in range(NBUF)]
    pbf = [A.bf([128, S]) for _ in range(NBUF)]; d_pbf = [Dep() for _ in range(NBUF)]
    pT = [A.bf([128, 16, 128]) for _ in range(NBUF)]; d_pT = [Dep() for _ in range(NBUF)]
    st1 = [A.f32([128, 4]) for _ in range(4)]
    d_mx1 = [Dep() for _ in range(4)]; d_ss1 = [Dep() for _ in range(4)]; d_gs1 = [Dep() for _ in range(4)]
    bufs = K.alloc_pn(256)
    identb = K.identb
    ovlb = K.ovlb
    RT = cc("RT")
    causal = cc("causal")
    winmask = cc("winmask")
    cmpmask = cc("cmpmask").rearrange("p (q c) -> p q c", q=16)
    anyvalid = cc("anyvalid")
    selmul = cc("selmul").rearrange("p (q n) -> p q n", q=16)
    seladd = cc("seladd").rearrange("p (q n) -> p q n", q=16)
    bk4, dbk4 = K.bank[4], K.dbank[4]
    bk5 = K.bank[5]
    d_po = [K.dbank[5]] * 3
    d_poc = K.dbank[5]
    d_pim = K.dbank[5]
    bi = 0
    ti_ = 0
    cnt = {"it": 0}

    def nb():
        nonlocal bi
        b = 4 + bi % 2; bi += 1
        if b == 5:
            return K.bank[5], [K.dbank[5]]
        return K.bank[4], [K.dbank[4]]

    def nT():
        nonlocal ti_
        b = ti_ % 2; ti_ += 1
        return K.psT[b], K.dpsT[b]

    def batch_pre(s, q2):
        tq = s * S + q2 * 256
        h2, dh2 = hb2[q2 % 2], d_hb2[q2 % 2]
        K.load_h_bf16(h2, src, tq, 256, dh2, "hb2", q2)
        c2, dc2 = cs2[q2 % 2], d_cs2[q2 % 2]
        P.dma("sp", lambda c2=c2, q2=q2: nc.sync.dma_start(out=c2, in_=K.cs_tab[:, :, q2 * 256:(q2 + 1) * 256].rearrange("two p t -> p two t")), sem("cs", q2, 2), writes=[dc2])
        for cp in range(8):
            pq, dpq = nb()
            pq = pq[:, 0:256]
            for kc_ in range(8):
                P.op("pe", lambda pq=pq, kc_=kc_, cp=cp, h2=h2: nc.tensor.matmul(pq, wq[:, kc_, cp, :], h2[:, kc_, :], start=(kc_ == 0), stop=(kc_ == 7)),
                     reads=d_wq + [dh2], writes=dpq)
            qw, dqw = qraw[cp % 2], d_qraw[cp % 2]
            q1, dq1 = qt1[cp % 2], d_qt1[cp % 2]
            P.op("act", lambda qw=qw, pq=pq: nc.scalar.mul(out=qw, in_=pq, mul=0.125), reads=dpq, writes=[dqw])
            P.op("act", lambda qw=qw, cp=cp: nc.scalar.copy(out=qb[:, cp, :], in_=qw), reads=[dqw], writes=[d_qb])
            pr, dpr = nb()
            pr = pr[:, 0:256]
            P.op("pe", lambda pr=pr, qw=qw: nc.tensor.matmul(pr, RT, qw, start=True, stop=True), reads=[dqw, d_cst], writes=dpr)
            P.op("dve", lambda q1=q1, qw=qw, c2=c2: nc.vector.tensor_tensor(out=q1, in0=qw, in1=c2[:, 0, :], op=ALU.mult), reads=[dqw, dc2], writes=[dq1])
            P.op("dve", lambda qw=qw, pr=pr, c2=c2: nc.vector.tensor_tensor(out=qw, in0=pr, in1=c2[:, 1, :], op=ALU.mult), reads=dpr + [dc2], writes=[dqw])
            P.op("dve", lambda q1=q1, qw=qw, cp=cp: nc.vector.tensor_tensor(out=qr[:, cp, :], in0=q1, in1=qw, op=ALU.add), reads=[dq1, dqw], writes=[d_qr])

    def tile_pre(s, q2, sub):
        h2, dh2 = hb2[q2 % 2], d_hb2[q2 % 2]
        ts = slice(sub * 128, (sub + 1) * 128)
        gts, d_gts = gts_[sub], d_gts_[sub]
        pg, dpg = nb()
        pg = pg[:, 0:48]
        for kc_ in range(8):
            P.op("pe", lambda pg=pg, kc_=kc_, h2=h2, ts=ts: nc.tensor.matmul(pg, h2[:, kc_, ts], wgt[:, kc_, :], start=(kc_ == 0), stop=(kc_ == 7)), reads=[dh2, d_wgt], writes=dpg)
        P.op("act", lambda pg=pg, gts=gts: nc.scalar.activation(out=gts, in_=pg, func=AF.Sigmoid), reads=dpg, writes=[d_gts])

    def tile_post(s, q2, sub):
        ts = slice(sub * 128, (sub + 1) * 128)
        oacc, d_oacc = oacc_[sub], d_oacc_[sub]
        oT, d_oT = oT_[q2 % 2], d_oT_[q2 % 2]
        P.op("act", lambda oacc=oacc: nc.scalar.copy(out=ob, in_=oacc), reads=d_oacc, writes=[d_ob])
        pt_, dpt_ = nT()
        for c in range(8):
            P.op("pe", lambda pt_=pt_, c=c: nc.tensor.transpose(pt_[:, c * 128:(c + 1) * 128], ob[:, c * 128:(c + 1) * 128], identb), reads=[d_ob, d_cst], writes=[dpt_])
        P.op("act", lambda pt_=pt_, ts=ts, oT=oT: nc.scalar.copy(out=oT[:, :, ts], in_=pt_.rearrange("p (c t) -> p c t", c=8)), reads=[dpt_], writes=[d_oT])

    def batch_post(s, q2):
        tq = s * S + q2 * 256
        oT, d_oT = oT_[q2 % 2], d_oT_[q2 % 2]

        def yfn(c):
            pb, dpb = nb()
            pb = pb[:, 0:256]
            for j in range(8):
                P.op("pe", lambda pb=pb, j=j, c=c: nc.tensor.matmul(pb, wo[:, j, c * 128:(c + 1) * 128], oT[:, j, :], start=(j == 0), stop=(j == 7)), reads=[d_wo, d_oT], writes=dpb)
            return pb, dpb
        K.postnorm_tile(src, dst, tq, 256, yfn, l, 0, bufs)

    def cmp_item(q2, sub, g):
        qt = q2 * 2 + sub
        ts = slice(sub * 128, (sub + 1) * 128)
        gp = g // 2
        base = (g % 2) * 64
        p = g % 2
        SC, d_SC = SC_[p], d_SC_[p]
        PCX, d_PCX = PCX_[p], d_PCX_[p]
        pn, d_pn = pn_[p], d_pn_[p]
        pTc, d_pTc = pTc_[p], d_pTc_[p]
        st4 = st4_[p]; d_mx4 = d_mx4_[p]; d_ss4 = d_ss4_[p]; d_rs4 = d_rs4_[p]
        score, top8, selb = score_[p], top8_[p], selb_[p]
        d_score, d_selb = d_score_[p], d_selb_[p]
        gts, d_gts = gts_[sub], d_gts_[sub]
        oacc, d_oacc = oacc_[sub], d_oacc_[sub]
        gts3 = gts.rearrange("p (h b) -> p h b", b=3)

        def stA():
            pc = bk4
            for hh in range(4):
                cp = gp * 4 + hh
                P.op("pe", lambda hh=hh, cp=cp: nc.tensor.matmul(pc[:, hh * 128:hh * 128 + 127], qb[base:base + 64, cp, ts], kc[base:base + 64, gp, 0:127], start=True, stop=True),
                     reads=[d_qb, d_kcl], writes=[dbk4])
            pc3 = pc.rearrange("p (h c) -> p h c", h=4)
            P.op("dve", lambda: nc.vector.tensor_tensor(out=SC[:, :, 0:127], in0=pc3[:, :, 0:127], in1=cmpmask[:, qt, 0:127].unsqueeze(1).to_broadcast([128, 4, 127]), op=ALU.add),
                 reads=[dbk4, d_cst], writes=[d_SC])
            P.op("dve", lambda: nc.vector.tensor_reduce(out=st4[:, 0:4], in_=SC[:, :, 0:127], axis=AX.X, op=ALU.max), reads=[d_SC], writes=[d_mx4])
            P.op("dve", lambda: nc.vector.tensor_scalar(out=st4[:, 4:8], in0=st4[:, 0:4], scalar1=-1.0, scalar2=None, op0=ALU.mult), reads=[d_mx4], writes=[d_mx4])

        def stB():
            for hh in range(4):
                P.op("act", lambda hh=hh: nc.scalar.activation(out=PCX[:, hh, 0:127], in_=SC[:, hh, 0:127], func=AF.Exp, bias=st4[:, 4 + hh:5 + hh], scale=1.0, accum_out=st4[:, 8 + hh:9 + hh]),
                     reads=[d_SC, d_mx4], writes=[d_PCX[hh], d_ss4[hh]])
            P.op("dve", lambda: nc.vector.reciprocal(out=st4[:, 12:16], in_=st4[:, 8:12]), reads=d_ss4, writes=[d_rs4])
            if qt == 0:
                P.op("dve", lambda: nc.vector.tensor_scalar(out=st4[:, 12:16], in0=st4[:, 12:16], scalar1=anyvalid[:, 0:1], scalar2=None, op0=ALU.mult), reads=[d_rs4, d_cst], writes=[d_rs4])
            P.op("dve", lambda: nc.vector.tensor_tensor(out=pn[:, :, 0:127], in0=PCX[:, :, 0:127], in1=st4[:, 12:16].unsqueeze(2).to_broadcast([128, 4, 127]), op=ALU.mult),
                 reads=d_PCX + [d_rs4], writes=[d_pn])

        def stB2():
            pt_, dpt_ = nT()
            for hh in range(4):
                P.op("pe", lambda pt_=pt_, hh=hh: nc.tensor.transpose(pt_[0:127, hh * 128:(hh + 1) * 128], pn[:, hh, 0:127], identb), reads=[d_pn, d_cst], writes=[dpt_])
            P.op("act", lambda pt_=pt_: nc.scalar.copy(out=pTc[0:127, :, :], in_=pt_[0:127, 0:512].rearrange("p (h t) -> p h t", h=4)), reads=[dpt_], writes=[d_pTc])

        def stC():
            po = bk5[:, 256:512]
            for hh in range(4):
                P.op("pe", lambda hh=hh: nc.tensor.matmul(po[:, hh * 64:(hh + 1) * 64], pTc[0:127, hh, :], vc[0:127, g * 64:(g + 1) * 64], start=True, stop=True),
                     reads=[d_pTc, d_vcl], writes=[d_poc])
            pim = bk5[:, 192:224]
            for hh in range(4):
                P.op("pe", lambda hh=hh: nc.tensor.matmul(pim, pTc[0:127, hh, :], ovlb[0:127, :], start=(hh == 0), stop=(hh == 3)), reads=[d_pTc, d_cst], writes=[d_pim])
            P.op("dve", lambda: nc.vector.tensor_tensor(out=oacc[:, g * 256:(g + 1) * 256].rearrange("p (h d) -> p h d", h=4), in0=po.rearrange("p (h d) -> p h d", h=4),
                                                        in1=gts3[:, g * 4:(g + 1) * 4, 0:1].to_broadcast([128, 4, 64]), op=ALU.mult),
                 reads=[d_poc, d_gts], writes=d_oacc[g * 4:(g + 1) * 4])
            P.op("dve", lambda: nc.vector.tensor_tensor(out=score, in0=pim, in1=selmul[:, qt, :], op=ALU.mult), reads=[d_pim, d_cst], writes=[d_score])
            P.op("dve", lambda: nc.vector.tensor_tensor(out=score, in0=score, in1=seladd[:, qt, :], op=ALU.add), reads=[d_score, d_cst], writes=[d_score])
            P.op("dve", lambda: nc.vector.max(out=top8, in_=score), reads=[d_score], writes=[d_score])
            P.op("dve", lambda: nc.vector.tensor_scalar(out=selb, in0=score, scalar1=top8[:, 7:8], scalar2=NEG, op0=ALU.is_lt, op1=ALU.mult), reads=[d_score], writes=[d_selb])
        return [stA, stB, stB2, stC]

    def att_item(q2, sub, g, hh, br):
        qt = q2 * 2 + sub
        t0 = qt * 128
        ts = slice(sub * 128, (sub + 1) * 128)
        gp = g // 2
        base = (g % 2) * 64
        h = g * 4 + hh
        cp = gp * 4 + hh
        k0 = 0 if br == 1 else max(0, t0 - 256)
        nkeys = t0 + 128 - k0
        it = cnt["it"]; cnt["it"] += 1
        sm, dsm = SM[it % NBUF], d_SM[it % NBUF]
        pb_, dpb_ = pbf[it % NBUF], d_pbf[it % NBUF]
        pTt, dpTt = pT[it % NBUF], d_pT[it % NBUF]
        stt = st1[it % 4]; dmx = d_mx1[it % 4]; dss = d_ss1[it % 4]; dgs = d_gs1[it % 4]
        selb, d_selb = selb_[g % 2], d_selb_[g % 2]
        gts, d_gts = gts_[sub], d_gts_[sub]
        oacc, d_oacc = oacc_[sub], d_oacc_[sub]
        nch = nkeys // 128
        pslot = it % 3

        def stA():
            if br == 1:
                nk5 = (nkeys + 511) // 512
                for kb in range(nk5):
                    n = min(512, nkeys - kb * 512)
                    P.op("pe", lambda kb=kb, n=n: nc.tensor.matmul(K.psS[:, kb * 512:kb * 512 + n], qr[base:base + 64, cp, ts], kT[base:base + 64, 0, gp, kb * 512:kb * 512 + n], start=True, stop=True),
                         reads=[d_qr, d_kTl], writes=[K.dbank[kb]])
                nblk = nkeys // 64
                P.op("dve", lambda: nc.vector.tensor_tensor(out=sm[:, 0:nkeys].rearrange("p (b k) -> p b k", k=64), in0=K.psS[:, 0:nkeys].rearrange("p (b k) -> p b k", k=64),
                                                            in1=selb[:, 0:nblk].unsqueeze(2).to_broadcast([128, nblk, 64]), op=ALU.add),
                     reads=list(K.dbank[0:nk5]) + [d_selb], writes=[dsm])
                P.op("dve", lambda: nc.vector.tensor_tensor(out=sm[:, t0:t0 + 128], in0=sm[:, t0:t0 + 128], in1=causal, op=ALU.add), reads=[dsm, d_cst], writes=[dsm])
            else:
                pw = bk4[:, 0:nkeys]
                P.op("pe", lambda: nc.tensor.matmul(pw, qr[base:base + 64, cp, ts], kT[base:base + 64, 1, gp, k0:k0 + nkeys], start=True, stop=True),
                     reads=[d_qr, d_kTl], writes=[dbk4])
                P.op("dve", lambda: nc.vector.tensor_tensor(out=sm[:, 0:nkeys], in0=pw, in1=winmask[:, 384 - nkeys:384], op=ALU.add), reads=[dbk4, d_cst], writes=[dsm])
            P.op("dve", lambda: nc.vector.reduce_max(out=stt[:, 0:1], in_=sm[:, 0:nkeys], axis=AX.X), reads=[dsm], writes=[dmx])
            P.op("dve", lambda: nc.vector.tensor_scalar(out=stt[:, 1:2], in0=stt[:, 0:1], scalar1=-1.0, scalar2=None, op0=ALU.mult), reads=[dmx], writes=[dmx])

        def stB():
            P.op("act", lambda: nc.scalar.activation(out=pb_[:, 0:nkeys], in_=sm[:, 0:nkeys], func=AF.Exp, bias=stt[:, 1:2], scale=1.0, accum_out=stt[:, 2:3]),
                 reads=[dsm, dmx], writes=[dpb_, dss])

        def stB2():
            for c0 in range(0, nch, 8):
                n8 = min(8, nch - c0)
                pt_, dpt_ = nT()
                for kk in range(n8):
                    P.op("pe", lambda pt_=pt_, kk=kk, c0=c0: nc.tensor.transpose(pt_[:, kk * 128:(kk + 1) * 128], pb_[:, (c0 + kk) * 128:(c0 + kk + 1) * 128], identb), reads=[dpb_, d_cst], writes=[dpt_])
                P.op("act", lambda pt_=pt_, c0=c0, n8=n8: nc.scalar.copy(out=pTt[:, c0:c0 + n8, :], in_=pt_[:, 0:n8 * 128].rearrange("p (c t) -> p c t", c=n8)), reads=[dpt_], writes=[dpTt])

        def stC():
            po = bk5[:, pslot * 64:(pslot + 1) * 64]
            dpo = d_po[pslot]
            kch0 = k0 // 128
            vi = 0 if br == 1 else 1
            for kk in range(nch):
                P.op("pe", lambda kk=kk: nc.tensor.matmul(po, pTt[:, kk, :], v[:, vi, kch0 + kk, g * 64:(g + 1) * 64], start=(kk == 0), stop=(kk == nch - 1)),
                     reads=[dpTt, d_vl], writes=[dpo])
            P.op("dve", lambda: nc.vector.reciprocal(out=stt[:, 3:4], in_=stt[:, 2:3]), reads=[dss], writes=[dgs])
            P.op("dve", lambda: nc.vector.tensor_tensor(out=stt[:, 3:4], in0=stt[:, 3:4], in1=gts[:, h * 3 + br:h * 3 + br + 1], op=ALU.mult), reads=[dgs, d_gts], writes=[dgs])
            P.op("dve", lambda: nc.vector.scalar_tensor_tensor(out=oacc[:, h * 64:(h + 1) * 64], in0=po, scalar=stt[:, 3:4], in1=oacc[:, h * 64:(h + 1) * 64], op0=ALU.mult, op1=ALU.add),
                 reads=[dpo, dgs, d_oacc[h]], writes=[d_oacc[h]])
        return [stA, stB, stB2, stC]

    for s in range(int(os.environ.get("NSA_DBG_NS", NS))):
        P.dma("sp", lambda s=s: nc.sync.dma_start(out=kT, in_=K.kvscr["kT"][s].rearrange("ti gp p k -> p ti gp k")), sem("kvl", 0, 4), writes=[d_kTl])
        P.dma("sp", lambda s=s: nc.sync.dma_start(out=v, in_=K.kvscr["v"][s].rearrange("ti p c n -> p ti c n")), sem("kvl", 1, 4), writes=[d_vl])
        P.dma("sp", lambda s=s: nc.sync.dma_start(out=kc, in_=K.kvscr["kc"][s].rearrange("gp p c -> p gp c")), sem("kvl", 2, 4), writes=[d_kcl])
        P.dma("sp", lambda s=s: nc.sync.dma_start(out=vc, in_=K.kvscr["vc"][s]), sem("kvl", 3, 4), writes=[d_vcl])
        entries = []
        for q2 in range(int(os.environ.get("NSA_DBG_Q2", 8))):
            for sub in range(2):
                order = [("c", 0)] + [("w", 0, hh) for hh in range(4)]
                for g in range(1, 4):
                    order += [("c", g)] + [("s", g - 1, hh) for hh in range(4)] + [("w", g, hh) for hh in range(4)]
                order += [("s", 3, hh) for hh in range(4)]
                for oi, od in enumerate(order):
                    pre = []
                    post = []
                    if oi == 0:
                        if sub == 0:
                            pre.append(lambda s=s, q2=q2: batch_pre(s, q2))
                        pre.append(lambda s=s, q2=q2, sub=sub: tile_pre(s, q2, sub))
                    if oi == len(order) - 1:
                        post.append(lambda s=s, q2=q2, sub=sub: tile_post(s, q2, sub))
                        if sub == 1:
                            post.append(lambda s=s, q2=q2: batch_post(s, q2))
                    if od[0] == "c":
                        mk = (lambda q2=q2, sub=sub, g=od[1]: cmp_item(q2, sub, g))
                    else:
                        mk = (lambda q2=q2, sub=sub, g=od[1], hh=od[2], br=(1 if od[0] == "s" else 2): att_item(q2, sub, g, hh, br))
                    entries.append((pre, mk, post))
        n = len(entries)
        stages = [None] * n
        for t in range(n + 3):
            if t < n:
                for f in entries[t][0]:
                    f()
                stages[t] = entries[t][1]()
                stages[t][0]()
            if 0 <= t - 1 < n:
                stages[t - 1][1]()
            if 0 <= t - 2 < n:
                stages[t - 2][2]()
            if 0 <= t - 3 < n:
                stages[t - 3][3]()
                for f in entries[t - 3][2]:
                    f()
                stages[t - 3] = None

_NC_CACHE = {}


def kernel(**inputs):
    stop = os.environ.get("KSTOP", "all")
    if stop not in _NC_CACHE:
        _NC_CACHE[stop] = build_program(stop)
    nc = _NC_CACHE[stop]
    x = np.asarray(inputs["x"], np.float32)
    sp = _pack_small(inputs)
    wmaps = {k: np.ascontiguousarray(np.asarray(inputs[k], np.float32)) for k in WSHAPES}
    in_maps = []
    for c in range(8):
        xs = x[2 * c:2 * c + 2].reshape(T, D)
        m = {"xT": np.ascontiguousarray(xs.T), "spk": sp, "cst": CST_NP, "cs_tab": CS_TAB}
        m.update(wmaps)
        in_maps.append(m)
    res = run_bass_kernel_spmd(nc, in_maps, core_ids=list(range(8)))
    out = np.empty((16, S, D), np.float32)
    for c in range(8):
        o = res.results[c]["outT"]
        out[2 * c:2 * c + 2] = o.T.reshape(2, S, D)
    return out
```
